# Optimizing a Trainium2 kernel written in Bass

```python
import jax, jax.numpy as jnp
from jax import lax
import numpy as np

D_MODEL = 1024
BATCH = 16
SEQ = 2048
DEPTH = 2

N_MEM = 256
D_MIX = D_MODEL
SB_HEADS = 8
SB_HEAD_DIM = (D_MIX // 2) // SB_HEADS
SB_WIDTH = SB_HEADS * SB_HEAD_DIM
SB_BLOCK = 128
RET_HEADS = 4
RET_HEAD_DIM = (D_MIX // 2) // RET_HEADS
RET_WIDTH = RET_HEADS * RET_HEAD_DIM
RET_CHUNK = 128
ROPE_BASE = 10000.0
IN_COLS = 3 * SB_WIDTH + 4 * RET_WIDTH
XA_HEADS = 4
XA_HEAD_DIM = D_MODEL // XA_HEADS
PEER_HEADS = 8
PEER_NKEYS = 128
PEER_N_EXPERTS = PEER_NKEYS * PEER_NKEYS
PEER_DQ = 256
PEER_TOPK = 16
PEER_CHUNK = 128
EPS = 1e-6

kernel_name = "hybrid_sb_retention_peer_trunk"


def rmsnorm(x, g):
    xf = x.astype(jnp.float32)
    y = xf * lax.rsqrt(jnp.mean(xf * xf, axis=-1, keepdims=True) + EPS)
    return (y * g.astype(jnp.float32)).astype(x.dtype)


def head_rmsnorm(o, g):
    H, d = o.shape[1], o.shape[3]
    of = o.astype(jnp.float32)
    y = of * lax.rsqrt(jnp.mean(of * of, axis=-1, keepdims=True) + EPS)
    return (y * g.reshape(1, H, 1, d).astype(jnp.float32)).astype(o.dtype)


def head_groupnorm(o, g):
    H, d = o.shape[1], o.shape[3]
    of = o.astype(jnp.float32)
    mu = jnp.mean(of, axis=-1, keepdims=True)
    var = jnp.mean((of - mu) ** 2, axis=-1, keepdims=True)
    y = (of - mu) * lax.rsqrt(var + EPS)
    return (y * g.reshape(1, H, 1, d).astype(jnp.float32)).astype(o.dtype)


def split_heads(t, n_heads):
    B, S, _ = t.shape
    return t.reshape(B, S, n_heads, -1).transpose(0, 2, 1, 3)


def merge_heads(t):
    B, H, S, d = t.shape
    return t.transpose(0, 2, 1, 3).reshape(B, S, H * d)


def rotary(x):
    S, d = x.shape[2], x.shape[3]
    inv = ROPE_BASE ** (-jnp.arange(0, d, 2, dtype=jnp.float32) / d)
    ang = jnp.arange(S, dtype=jnp.float32)[:, None] * inv[None, :]
    cos, sin = jnp.cos(ang), jnp.sin(ang)
    xf = x.astype(jnp.float32)
    x1, x2 = xf[..., : d // 2], xf[..., d // 2:]
    return jnp.concatenate([x1 * cos - x2 * sin, x1 * sin + x2 * cos], axis=-1).astype(x.dtype)


def stick_breaking(q, k, v):
    S, d = q.shape[2], q.shape[3]
    scale = d ** -0.5
    outs = []
    for blk in range(S // SB_BLOCK):
        t0, t1 = blk * SB_BLOCK, (blk + 1) * SB_BLOCK
        z = jnp.einsum('bhtd,bhsd->bhts', q[:, :, t0:t1], k[:, :, :t1]).astype(jnp.float32) * scale
        causal = jnp.arange(t1)[None, :] < jnp.arange(t0, t1)[:, None]
        log_not_b = jnp.where(causal, jax.nn.log_sigmoid(-z), 0.0)
        stick = lax.cumsum(log_not_b, axis=3, reverse=True) - log_not_b
        a = jnp.where(causal, jnp.exp(jax.nn.log_sigmoid(z) + stick), 0.0)
        outs.append(jnp.einsum('bhts,bhsd->bhtd', a.astype(v.dtype), v[:, :, :t1]))
    return jnp.concatenate(outs, axis=2)


def retention(q, k, v):
    B, H, S, d = q.shape
    C = RET_CHUNK
    N = S // C
    log_g = jnp.log1p(-jnp.power(2.0, -5.0 - jnp.arange(H, dtype=jnp.float32)))
    qc = q.astype(jnp.float32).reshape(B, H, N, C, d)
    kc = (k.astype(jnp.float32) * d ** -0.5).reshape(B, H, N, C, d)
    vc = v.astype(jnp.float32).reshape(B, H, N, C, v.shape[3])
    idx = jnp.arange(C, dtype=jnp.float32)
    diff = idx[:, None] - idx[None, :]
    decay = jnp.where(diff >= 0, jnp.exp(diff[None] * log_g[:, None, None]), 0.0)
    inner_s = jnp.einsum('bhncd,bhnmd->bhncm', qc, kc) * decay[None, :, None]
    inner = jnp.einsum('bhncm,bhnme->bhnce', inner_s, vc)
    zeta = jnp.exp((C - 1 - idx)[None, :] * log_g[:, None])
    xi = jnp.exp((idx + 1)[None, :] * log_g[:, None])
    chunk_kv = jnp.einsum('bhncd,bhnce->nbhde', kc * zeta[None, :, None, :, None], vc)
    decay_c = jnp.exp(C * log_g)[None, :, None, None]

    def step(state, kv_n):
        return decay_c * state + kv_n, state

    _, prev = lax.scan(step, jnp.zeros_like(chunk_kv[0]), chunk_kv)
    cross = jnp.einsum('bhncd,nbhde->bhnce', qc * xi[None, :, None, :, None], prev)
    return (inner + cross).reshape(B, H, S, v.shape[3]).astype(q.dtype)


def head_group_mixer(h, w_in, sb_norm, ret_norm, w_out):
    proj = h @ w_in
    cuts = [SB_WIDTH, 2 * SB_WIDTH, 3 * SB_WIDTH, 3 * SB_WIDTH + RET_WIDTH,
            3 * SB_WIDTH + 2 * RET_WIDTH, 3 * SB_WIDTH + 3 * RET_WIDTH]
    sb_q, sb_k, sb_v, r_q, r_k, r_v, r_g = jnp.split(proj, cuts, axis=-1)
    sb_o = stick_breaking(split_heads(sb_q, SB_HEADS), split_heads(sb_k, SB_HEADS),
                          split_heads(sb_v, SB_HEADS))
    sb_o = merge_heads(head_rmsnorm(sb_o, sb_norm))
    ret_o = retention(rotary(split_heads(r_q, RET_HEADS)), rotary(split_heads(r_k, RET_HEADS)),
                      split_heads(r_v, RET_HEADS))
    ret_o = merge_heads(head_groupnorm(ret_o, ret_norm)) * jax.nn.silu(r_g)
    return jnp.concatenate([sb_o, ret_o], axis=-1) @ w_out


def memory_cross_attn(h, mem_h, wq, wkv, wo):
    B, S, D = h.shape
    M = mem_h.shape[1]
    q = (h @ wq).reshape(B, S, XA_HEADS, XA_HEAD_DIM)
    k, v = jnp.split(mem_h @ wkv, 2, axis=-1)
    k = k.reshape(B, M, XA_HEADS, XA_HEAD_DIM)
    v = v.reshape(B, M, XA_HEADS, XA_HEAD_DIM)
    s = jnp.einsum('bshd,bmhd->bhsm', q, k).astype(jnp.float32) * XA_HEAD_DIM ** -0.5
    p = jax.nn.softmax(s, axis=-1).astype(v.dtype)
    o = jnp.einsum('bhsm,bmhd->bshd', p, v).reshape(B, S, D)
    return o @ wo


def peer_ffn(h, w_pq, sub_keys, expert_u, expert_v):
    B, S, D = h.shape
    K, NK = PEER_TOPK, PEER_NKEYS
    q = (h @ w_pq).reshape(B, S, PEER_HEADS, 2, PEER_DQ // 2)
    scores = jnp.einsum('bshpd,hpkd->bshpk', q, sub_keys).astype(jnp.float32)
    top_s, top_i = lax.top_k(scores, K)
    cand_s = (top_s[..., 0, :, None] + top_s[..., 1, None, :]).reshape(B, S, PEER_HEADS, K * K)
    cand_i = (top_i[..., 0, :, None] * NK + top_i[..., 1, None, :]).reshape(B, S, PEER_HEADS, K * K)
    best_s, best_pos = lax.top_k(cand_s, K)
    expert_idx = jnp.take_along_axis(cand_i, best_pos, axis=-1)
    gate = jax.nn.softmax(best_s, axis=-1).astype(h.dtype)
    T = B * S
    n_chunks = T // PEER_CHUNK
    h_c = h.reshape(n_chunks, PEER_CHUNK, D)
    i_c = expert_idx.reshape(n_chunks, PEER_CHUNK, PEER_HEADS, K)
    g_c = gate.reshape(n_chunks, PEER_CHUNK, PEER_HEADS, K)

    def retrieve(args):
        hc, ic, gc = args
        u = jnp.take(expert_u, ic, axis=0)
        a = jnp.einsum('cd,chkd->chk', hc, u)
        w = gc * jax.nn.gelu(a, approximate=False)
        return jnp.einsum('chk,chkd->cd', w, jnp.take(expert_v, ic, axis=0))

    return lax.map(retrieve, (h_c, i_c, g_c)).reshape(B, S, D)


def setup_inputs(seed: int = 0) -> dict:
    key = jax.random.key(seed)
    ks = jax.random.split(key, 20)
    f32 = jnp.float32

    def nrm(k, shape, scale):
        return jax.random.normal(k, shape, f32) * scale

    def gain(k, shape):
        return 1.0 + 0.01 * jax.random.normal(k, shape, f32)

    L, D = DEPTH, D_MODEL
    return {
        "x": nrm(ks[0], (BATCH, SEQ, D), 1.0),
        "mem": nrm(ks[1], (BATCH, N_MEM, D), 1.0),
        "mix_norm": gain(ks[2], (L, D)),
        "w_in": nrm(ks[3], (L, D, IN_COLS), D ** -0.5),
        "sb_norm": gain(ks[4], (L, SB_WIDTH)),
        "ret_norm": gain(ks[5], (L, RET_WIDTH)),
        "w_out": nrm(ks[6], (L, D_MIX, D), 0.5 * D_MIX ** -0.5),
        "xa_norm": gain(ks[7], (L, D)),
        "mem_norm": gain(ks[8], (L, D)),
        "xa_wq": nrm(ks[9], (L, D, D), D ** -0.5),
        "xa_wkv": nrm(ks[10], (L, D, 2 * D), D ** -0.5),
        "xa_wo": nrm(ks[11], (L, D, D), 0.5 * D ** -0.5),
        "peer_norm": gain(ks[12], (L, D)),
        "peer_wq": nrm(ks[13], (L, D, PEER_HEADS * PEER_DQ), D ** -0.5),
        "peer_keys": nrm(ks[14], (L, PEER_HEADS, 2, PEER_NKEYS, PEER_DQ // 2), (PEER_DQ // 2) ** -0.5),
        "peer_u": nrm(ks[15], (L, PEER_N_EXPERTS, D), D ** -0.5),
        "peer_v": nrm(ks[16], (L, PEER_N_EXPERTS, D), 0.25),
        "final_norm": gain(ks[17], (D,)),
    }


def reference(x, mem, mix_norm, w_in, sb_norm, ret_norm, w_out, xa_norm, mem_norm,
              xa_wq, xa_wkv, xa_wo, peer_norm, peer_wq, peer_keys, peer_u, peer_v, final_norm):
    for l in range(DEPTH):
        x = x + head_group_mixer(rmsnorm(x, mix_norm[l]), w_in[l], sb_norm[l], ret_norm[l], w_out[l])
        x = x + memory_cross_attn(rmsnorm(x, xa_norm[l]), rmsnorm(mem, mem_norm[l]),
                                  xa_wq[l], xa_wkv[l], xa_wo[l])
        x = x + peer_ffn(rmsnorm(x, peer_norm[l]), peer_wq[l], peer_keys[l], peer_u[l], peer_v[l])
    return rmsnorm(x, final_norm)
```

```python
import contextlib
import numpy as np
import concourse.bass as bass
import concourse.mybir as mybir
from concourse.bass_utils import run_bass_kernel_spmd

F32 = mybir.dt.float32
BF16 = mybir.dt.bfloat16
U32 = mybir.dt.uint32
I32 = mybir.dt.int32
AF = mybir.ActivationFunctionType
ALU = mybir.AluOpType
AX = mybir.AxisListType

S = 2048
NT = 16
D = 1024
DC = 8
NM = 256
L = 2
NSEQ = 2
EPS = 1e-6
INC = 3584
NEXP = 16384
UG = 256
NUG = NEXP // UG
TB = 256


class Eng:
    def __init__(self, k, name, h, same_sync):
        self.name = name
        self.h = h
        self.sem = k.new_sem("e_" + name)
        self.count = 0
        self.seq = 0
        self.sigmap = {}
        self.pending = []
        self.seen = {}
        self.same_sync = same_sync


class DSem:
    def __init__(self, h):
        self.h = h
        self.count = 0


class Tile:
    __slots__ = ("ap", "name", "last_w", "readers", "dsem", "excl", "bf")

    def __init__(self, ap, name="", excl=False):
        self.ap = ap
        self.name = name
        self.last_w = None
        self.readers = {}
        self.dsem = None
        self.excl = excl
        self.bf = None

    def __getitem__(self, idx):
        return self.ap[idx]


class K:
    def __init__(self, nc, stack):
        self.nc = nc
        self.stack = stack
        self.pe = Eng(self, "pe", nc.tensor, False)
        self.act = Eng(self, "act", nc.scalar, True)
        self.dve = Eng(self, "dve", nc.vector, True)
        self.pool = Eng(self, "pool", nc.gpsimd, True)
        self.sp = Eng(self, "sp", nc.sync, False)
        self.engs = [self.pe, self.act, self.dve, self.pool, self.sp]
        self.dsems = []
        self.dsem_by_name = {}

    def new_sem(self, name):
        return self.stack.enter_context(self.nc.semaphore(name))

    def get_dsem(self, name):
        if name in self.dsem_by_name:
            return self.dsem_by_name[name]
        d = DSem(self.new_sem("d%d" % len(self.dsems)))
        self.dsems.append(d)
        self.dsem_by_name[name] = d
        return d

    def _resolve(self, eng, dep):
        if dep[0] == "e":
            src, seq = dep[1], dep[2]
            if src is eng and not eng.same_sync:
                return None
            cnt = src.sigmap.get(seq)
            if cnt is None:
                if src is eng:
                    return None
                raise RuntimeError("dependency on unsignaled instruction of %s" % src.name)
            return (src.sem, id(src), cnt)
        d, cnt = dep[1], dep[2]
        return (d.h, id(d), cnt)

    def _wait(self, eng, deps):
        best = {}
        for dep in deps:
            r = self._resolve(eng, dep)
            if r is None:
                continue
            sem, key, cnt = r
            if eng.seen.get(key, 0) >= cnt:
                continue
            if key not in best or best[key][1] < cnt:
                best[key] = (sem, cnt)
        for key, (sem, cnt) in best.items():
            eng.h.wait_ge(sem, cnt)
            eng.seen[key] = cnt

    @staticmethod
    def _gather(R, W):
        deps = []
        for t in R:
            if t.last_w is not None:
                deps.append(t.last_w)
        for t in W:
            if t.last_w is not None:
                deps.append(t.last_w)
            deps.extend(t.readers.values())
        return deps

    def op(self, eng, fn, R=(), W=(), sig=True, indep=False):
        ex = [t for t in R if t.excl]
        if ex:
            W = list(W) + [t for t in ex if t not in W]
        deps = self._gather(R, W)
        if indep:
            deps = [d for d in deps if not (d[0] == "e" and d[1] is eng)]
        self._wait(eng, deps)
        ins = fn()
        eng.seq += 1
        seq = eng.seq
        if sig:
            eng.count += 1
            ins.then_inc(eng.sem, 1)
            for s in eng.pending:
                eng.sigmap[s] = eng.count
            eng.pending = []
            eng.sigmap[seq] = eng.count
        else:
            eng.pending.append(seq)
        me = ("e", eng, seq)
        for t in R:
            t.readers[id(eng)] = me
        for t in W:
            t.last_w = me
            t.readers = {}
        return ins

    def dma(self, q, out, in_, R=(), W=(), semtile=None, **kw):
        self._wait(q, self._gather(R, W))
        if semtile is None:
            semtile = W[0] if len(W) else R[0]
        if semtile.dsem is None:
            semtile.dsem = self.get_dsem(semtile.name)
        d = semtile.dsem
        ins = q.h.dma_start(out=out, in_=in_, **kw)
        ins.then_inc(d.h, 16)
        d.count += 16
        me = ("d", d, d.count)
        for t in R:
            t.readers[id(d)] = me
        for t in W:
            t.last_w = me
            t.readers = {}
        return ins

    def barrier(self):
        for e in self.engs:
            if e.pending:
                raise RuntimeError("barrier with unsignaled pending instructions on " + e.name)
        for e in self.engs:
            for o in self.engs:
                if o.count == 0 or (o is e and not e.same_sync):
                    continue
                if e.seen.get(id(o), 0) < o.count:
                    e.h.wait_ge(o.sem, o.count)
                    e.seen[id(o)] = o.count
            for d in self.dsems:
                if d.count and e.seen.get(id(d), 0) < d.count:
                    e.h.wait_ge(d.h, d.count)
                    e.seen[id(d)] = d.count


def esize(dt):
    return 4 if dt in (F32, U32, I32) else 2


class Region:
    def __init__(self, arena, lo, hi):
        self.arena = arena
        self.lo = lo
        self.hi = hi
        self.off = lo

    def reset(self):
        self.off = self.lo

    def alloc(self, shape_free, dtype, name=""):
        n = int(np.prod(shape_free))
        nb = (n * esize(dtype) + 31) // 32 * 32
        if self.off + nb > self.hi:
            raise RuntimeError("region overflow %s: need %d at %d (hi %d)" % (name, nb, self.off, self.hi))
        a = self.arena[:, self.off // 2:(self.off + n * esize(dtype)) // 2]
        self.off += nb
        if dtype != BF16:
            a = a.bitcast(dtype)
        if len(shape_free) == 2:
            a = a.rearrange("p (a b) -> p a b", a=shape_free[0])
        elif len(shape_free) == 3:
            a = a.rearrange("p (a b c) -> p a b c", a=shape_free[0], b=shape_free[1])
        return Tile(a, name)


def run_chains(chains, width):
    chains = list(chains)
    active = []
    nxt = 0
    while nxt < len(chains) or active:
        while len(active) < width and nxt < len(chains):
            active.append(chains[nxt]())
            nxt += 1
        for g in list(active):
            try:
                next(g)
            except StopIteration:
                active.remove(g)


KB = 1024


class Builder:
    def __init__(self, nc, stack, cfg):
        self.nc = nc
        self.cfg = cfg
        self.k = K(nc, stack)
        k = self.k
        dt = nc.dram_tensor
        nl = cfg["L"]
        nseq = cfg["NSEQ"]
        self.nl, self.nseq = nl, nseq
        self.x_d = dt("x", [nseq, S, D], F32, kind="ExternalInput").ap()
        self.mem_d = dt("mem", [nseq, NM, D], F32, kind="ExternalInput").ap()
        self.out_d = dt("out", [nseq, S, D], F32, kind="ExternalOutput").ap()
        self.w_in_d = dt("w_in", [nl, D, INC], F32, kind="ExternalInput").ap()
        self.w_out_d = dt("w_out", [nl, D, D], F32, kind="ExternalInput").ap()
        self.gcols_d = dt("gcols", [128, 40 * nl], F32, kind="ExternalInput").ap()
        self.fing_d = dt("fing", [128, D], F32, kind="ExternalInput").ap()
        self.cos_d = dt("cos", [128, NT, 64], F32, kind="ExternalInput").ap()
        self.sin_d = dt("sin", [128, NT, 64], F32, kind="ExternalInput").ap()
        self.dq_d = dt("dq", [128, NT, 4], F32, kind="ExternalInput").ap()
        self.dk_d = dt("dk", [128, NT, 4], F32, kind="ExternalInput").ap()
        self.xa_wq_d = dt("xa_wq", [nl, D, D], F32, kind="ExternalInput").ap()
        self.xa_wkv_d = dt("xa_wkv", [nl, D, 2 * D], F32, kind="ExternalInput").ap()
        self.xa_wo_d = dt("xa_wo", [nl, D, D], F32, kind="ExternalInput").ap()
        self.xa_wq_b = [Tile(dt("xa_wq_b%d" % l_, [D, D], BF16, kind="Internal").ap(), "xa_wq_b%d" % l_) for l_ in range(nl)]
        self.xa_wkv_b = [Tile(dt("xa_wkv_b%d" % l_, [D, 2 * D], BF16, kind="Internal").ap(), "xa_wkv_b%d" % l_) for l_ in range(nl)]
        self.xa_wo_b = [Tile(dt("xa_wo_b%d" % l_, [D, D], BF16, kind="Internal").ap(), "xa_wo_b%d" % l_) for l_ in range(nl)]
        self.peer_wq_d = dt("peer_wq", [nl, D, 2048], F32, kind="ExternalInput").ap()
        self.keysT_d = dt("keysT", [nl, 128, 16 * 128], F32, kind="ExternalInput").ap()
        self.UT_d = dt("UT", [nl, NUG, 128, DC * UG], F32, kind="ExternalInput").ap()
        self.V_d = dt("V", [nl, NEXP, D], F32, kind="ExternalInput").ap()
        self.peer_wq_b = [Tile(dt("peer_wq_b%d" % l_, [D, 2048], BF16, kind="Internal").ap(), "peer_wq_b%d" % l_) for l_ in range(nl)]
        self.keysT_b = [Tile(dt("keysT_b%d" % l_, [128, 16 * 128], BF16, kind="Internal").ap(), "keysT_b%d" % l_) for l_ in range(nl)]
        self.UV_b = [Tile(dt("UV_b%d" % l, [NUG, 128, 4096], BF16, kind="Internal").ap(), "UV_b%d" % l) for l in range(nl)]
        self.w_in_b = [Tile(dt("w_in_b%d" % l_, [D, INC], BF16, kind="Internal").ap(), "w_in_b%d" % l_) for l_ in range(nl)]
        self.w_out_b = [Tile(dt("w_out_b%d" % l_, [D, D], BF16, kind="Internal").ap(), "w_out_b%d" % l_) for l_ in range(nl)]
        if cfg.get("dbg"):
            self.dbg_ret = dt("dbg_ret", [128, NT, 512], BF16, kind="ExternalOutput").ap()
            self.dbg_sb = dt("dbg_sb", [128, NT, 512], BF16, kind="ExternalOutput").ap()
        sb = lambda name, shape, dtp: stack.enter_context(nc.sbuf_tensor(name, shape, dtp))
        xs = sb("xs", [128, NT, D], F32)
        self.x = [Tile(xs[:, t, :], "x%d" % t) for t in range(NT)]
        self.ident_f = Tile(sb("ident_f", [128, 128], F32)[:], "ident_f")
        self.ident_b = Tile(sb("ident_b", [128, 128], BF16)[:], "ident_b")
        self.ones_b = Tile(sb("ones_b", [128, 128], BF16)[:], "ones_b")
        self.tri_incl = Tile(sb("tri_incl", [128, 128], BF16)[:], "tri_incl")
        self.tri_low = Tile(sb("tri_low", [128, 128], BF16)[:], "tri_low")
        self.gcols = Tile(sb("gcols_sb", [128, 40 * nl], F32)[:], "gcols")
        self.iota_i = Tile(sb("iota_i", [128, 128], I32)[:], "iota_i")
        self.iota_f = Tile(sb("iota_f", [128, 128], F32)[:], "iota_f")
        self.iota_b = Tile(sb("iota_b", [128, 128], BF16)[:], "iota_b")
        self.dq = Tile(sb("dq_sb", [128, NT, 4], F32)[:], "dq")
        self.dk = Tile(sb("dk_sb", [128, NT, 4], F32)[:], "dk")
        self.ARENA = 136 * KB
        self.arena = sb("arena", [128, self.ARENA // 2], BF16)
        self.banks = [stack.enter_context(nc.psum_tensor("bank%d" % i, [128, 512], F32)) for i in range(8)]

    def region(self, lo_kb, hi_kb):
        return Region(self.arena, int(lo_kb * KB), int(hi_kb * KB))

    def psum(self, i, name=""):
        return Tile(self.banks[i][:], name or ("bank%d" % i))

    def psum_bf(self, i, name=""):
        return Tile(self.banks[i][:].bitcast(BF16), name or ("bankbf%d" % i))

    def setup(self):
        k, nc = self.k, self.nc
        k.op(k.pool, lambda: nc.gpsimd.memset(self.ident_f[:], 0.0), W=[self.ident_f])
        k.op(k.pool, lambda: nc.gpsimd.affine_select(out=self.ident_f[:], in_=self.ident_f[:], pattern=[[-1, 128]],
                                                     compare_op=ALU.not_equal, fill=1.0, base=0, channel_multiplier=1),
             R=[self.ident_f], W=[self.ident_f])
        k.op(k.dve, lambda: nc.vector.tensor_copy(out=self.ident_b[:], in_=self.ident_f[:]), R=[self.ident_f], W=[self.ident_b])
        k.op(k.pool, lambda: nc.gpsimd.memset(self.ones_b[:], 1.0), W=[self.ones_b])
        k.op(k.pool, lambda: nc.gpsimd.memset(self.tri_incl[:], 1.0), W=[self.tri_incl])
        k.op(k.pool, lambda: nc.gpsimd.affine_select(out=self.tri_incl[:], in_=self.tri_incl[:], pattern=[[-1, 128]],
                                                     compare_op=ALU.is_ge, fill=0.0, base=0, channel_multiplier=1),
             R=[self.tri_incl], W=[self.tri_incl])
        k.op(k.pool, lambda: nc.gpsimd.memset(self.tri_low[:], 1.0), W=[self.tri_low])
        k.op(k.pool, lambda: nc.gpsimd.affine_select(out=self.tri_low[:], in_=self.tri_low[:], pattern=[[1, 128]],
                                                     compare_op=ALU.is_gt, fill=0.0, base=0, channel_multiplier=-1),
             R=[self.tri_low], W=[self.tri_low])
        k.op(k.pool, lambda: nc.gpsimd.iota(self.iota_i[:], pattern=[[1, 128]], base=0, channel_multiplier=0), W=[self.iota_i])
        k.op(k.dve, lambda: nc.vector.tensor_copy(out=self.iota_f[:], in_=self.iota_i[:]), R=[self.iota_i], W=[self.iota_f])
        k.op(k.dve, lambda: nc.vector.tensor_copy(out=self.iota_b[:], in_=self.iota_i[:]), R=[self.iota_i], W=[self.iota_b])
        for t, d in ((self.gcols, self.gcols_d), (self.dq, self.dq_d), (self.dk, self.dk_d)):
            k.dma(k.sp, t[:], d, W=[t], semtile=self.gcols)
        for l in range(self.nl):
            for r0 in range(0, D, 256):
                k.dma(k.pool, self.w_in_b[l][r0:r0 + 256, :], self.w_in_d[l, r0:r0 + 256, :], W=[self.w_in_b[l]])
            for r0 in range(0, D, 512):
                k.dma(k.pool, self.w_out_b[l][r0:r0 + 512, :], self.w_out_d[l, r0:r0 + 512, :], W=[self.w_out_b[l]])
            for r0 in range(0, D, 512):
                k.dma(k.pool, self.xa_wq_b[l][r0:r0 + 512, :], self.xa_wq_d[l, r0:r0 + 512, :], W=[self.xa_wq_b[l]])
            for r0 in range(0, D, 256):
                k.dma(k.pool, self.xa_wkv_b[l][r0:r0 + 256, :], self.xa_wkv_d[l, r0:r0 + 256, :], W=[self.xa_wkv_b[l]])
            for r0 in range(0, D, 512):
                k.dma(k.pool, self.xa_wo_b[l][r0:r0 + 512, :], self.xa_wo_d[l, r0:r0 + 512, :], W=[self.xa_wo_b[l]])
            if "peer" in self.cfg["phases"]:
                for r0 in range(0, D, 256):
                    k.dma(k.pool, self.peer_wq_b[l][r0:r0 + 256, :], self.peer_wq_d[l, r0:r0 + 256, :], W=[self.peer_wq_b[l]])
                k.dma(k.pool, self.keysT_b[l][:], self.keysT_d[l], W=[self.keysT_b[l]])
                for g0 in range(0, NUG, 8):
                    k.dma(k.pool, self.UV_b[l][g0:g0 + 8, :, 0:2048].rearrange("g p n -> p g n"),
                          self.UT_d[l, g0:g0 + 8].rearrange("g p n -> p g n"), W=[self.UV_b[l]])
                    for cc in range(2):
                        k.dma(k.pool, self.UV_b[l][g0:g0 + 8, :, 2048 + cc * 1024:2048 + (cc + 1) * 1024].rearrange("g j d -> j g d"),
                              self.V_d[l, g0 * UG:(g0 + 8) * UG, :].rearrange("(g cc j) d -> j g cc d", g=8, j=128, cc=2)[:, :, cc, :],
                              W=[self.UV_b[l]])

    def load_x(self, b):
        k = self.k
        for t in range(NT):
            k.dma(k.sp, self.x[t][:], self.x_d[b, t * 128:(t + 1) * 128, :], W=[self.x[t]])

    def store(self, b, dst):
        k = self.k
        for t in range(NT):
            k.dma(k.sp, dst[b, t * 128:(t + 1) * 128, :], self.x[t][:], R=[self.x[t]])

    def norm_act(self, src_tile, src_ap, wk):
        k, nc = self.k, self.nc
        junk, ss, lnv, rstd, hn = wk
        k.op(k.act, lambda: nc.scalar.activation(out=junk[:], in_=src_ap, func=AF.Square, accum_out=ss[:]),
             R=[src_tile], W=[junk, ss])
        k.op(k.act, lambda: nc.scalar.activation(out=lnv[:], in_=ss[:], func=AF.Ln, scale=1.0 / D, bias=self.eps_t[:]),
             R=[ss, self.eps_t], W=[lnv])
        k.op(k.act, lambda: nc.scalar.activation(out=rstd[:], in_=lnv[:], func=AF.Exp, scale=-0.5), R=[lnv], W=[rstd])
        k.op(k.act, lambda: nc.scalar.activation(out=hn[:], in_=src_ap, func=AF.Copy, scale=rstd[:]),
             R=[src_tile, rstd], W=[hn])

    def norm_tr(self, gbase, dst_tile, dst_ap3, wk, bank):
        k, nc = self.k, self.nc
        hn = wk[4]
        pT = self.ps[bank]
        for c in range(DC):
            k.op(k.pe, lambda c=c: nc.tensor.transpose(out=pT.bf[:, c * 128:(c + 1) * 128], in_=hn[:, c * 128:(c + 1) * 128],
                                                       identity=self.ident_b[:]),
                 R=[hn, self.ident_b], W=[pT], sig=(c == DC - 1))
        gc = self.gcols[:, gbase:gbase + DC].unsqueeze(2).broadcast_to([128, DC, 128])
        k.op(k.dve, lambda: nc.vector.tensor_tensor(out=dst_ap3, in0=pT.bf.rearrange("p (c t) -> p c t", c=DC), in1=gc,
                                                    op=ALU.mult),
             R=[pT, self.gcols], W=[dst_tile])

    def norm_T(self, src_tile, src_ap, gbase, dst_tile, dst_ap3, wk, bank):
        self.norm_act(src_tile, src_ap, wk)
        self.norm_tr(gbase, dst_tile, dst_ap3, wk, bank)

    def new_phase(self):
        self.k.barrier()
        self.ps = []
        for i in range(8):
            t = Tile(self.banks[i][:], "bank%d" % i, excl=True)
            t.bf = self.banks[i][:].bitcast(BF16)
            self.ps.append(t)

    def mixer(self, l):
        k, nc = self.k, self.nc
        gb = 40 * l
        R0 = self.region(0, 32)
        R1 = self.region(32, 80)
        R2 = self.region(80, 96)
        R3 = self.region(96, 112)
        R4 = self.region(112, 122)
        R5 = self.region(122, 130)
        self.new_phase()
        self.cos = R5.alloc([NT, 64], F32, "cos")
        self.sin = R5.alloc([NT, 64], F32, "sin")
        k.dma(k.sp, self.cos[:], self.cos_d, W=[self.cos])
        k.dma(k.sp, self.sin[:], self.sin_d, W=[self.sin])
        hT = [R0.alloc([DC, 512], BF16, "hT%d" % g) for g in range(4)]
        R4.reset()
        self.eps_t = R4.alloc([1], F32, "eps")
        k.op(k.dve, lambda: nc.vector.memset(self.eps_t[:], EPS), W=[self.eps_t])
        wks = [(R4.alloc([D], BF16, "junk"), R4.alloc([1], F32, "ss"), R4.alloc([1], F32, "lnv"), R4.alloc([1], F32, "rstd"),
                R4.alloc([D], BF16, "hn")) for _ in range(2)]
        for t in range(NT):
            g, tl = divmod(t, 4)
            self.norm_T(self.x[t], self.x[t][:], gb + 0, hT[g], hT[g][:, :, tl * 128:(tl + 1) * 128], wks[t % 2], t % 2)
        if self.cfg.get('mx_stop', 99) <= 1:
            return
        self.new_phase()
        R4.reset()
        rqT = R1.alloc([4, S], BF16, "rqT")
        rkT = R1.alloc([4, S], BF16, "rkT")
        r_v = R1.alloc([NT, 512], BF16, "r_v")
        ret_n = R2.alloc([NT, 512], BF16, "ret_n")
        wbuf = [R3.alloc([DC, 512], BF16, "wbuf%d" % i) for i in range(2)]
        nwb = [0]

        def load_w(col0):
            wb = wbuf[nwb[0] % 2]
            nwb[0] += 1
            k.dma(k.sp, wb[:], self.w_in_b[l][:, col0:col0 + 512].rearrange("(c p) n -> p c n", p=128), R=[self.w_in_b[l]], W=[wb])
            return wb

        def proj_tm(wb, t, bank):
            g, tl = divmod(t, 4)
            ps = self.ps[bank]
            for c in range(DC):
                k.op(k.pe, lambda c=c: nc.tensor.matmul(ps[:], lhsT=hT[g][:, c, tl * 128:(tl + 1) * 128], rhs=wb[:, c, :],
                                                        start=(c == 0), stop=(c == DC - 1)),
                     R=[hT[g], wb], W=[ps], sig=(c == DC - 1))
            return ps

        rot_tmp = [R4.alloc([4, 2, 64], F32, "rot_tmp%d" % i) for i in range(2)]
        rot_a = R4.alloc([4, 64], F32, "rot_a")
        rot_b = R4.alloc([4, 64], F32, "rot_b")
        rot_o = [R4.alloc([4, 128], BF16, "rot_o%d" % i) for i in range(2)]
        def emit_tr(t, dstT):
            ro = rot_o[t % 2]
            pT = self.ps[4 + t % 2]
            for h in range(4):
                k.op(k.pe, lambda h=h: nc.tensor.transpose(out=pT.bf[:, h * 128:(h + 1) * 128], in_=ro[:, h, :],
                                                           identity=self.ident_b[:]),
                     R=[ro, self.ident_b], W=[pT], sig=(h == 3))
            k.op(k.act, lambda: nc.scalar.copy(out=dstT[:, :, t * 128:(t + 1) * 128],
                                               in_=pT.bf[:, 0:512].rearrange("p (h t) -> p h t", h=4)),
                 R=[pT], W=[dstT])

        for which, (col0, dstT, dec) in enumerate(((1536, rqT, self.dq), (2048, rkT, self.dk))):
            wb = load_w(col0)
            pss = {0: proj_tm(wb, 0, 2)}
            for t in range(NT):
                if t + 1 < NT:
                    pss[t + 1] = proj_tm(wb, t + 1, 2 + (t + 1) % 2)
                ps = pss.pop(t)
                v = ps[:].rearrange("p (h two d) -> p h two d", h=4, two=2)
                x1, x2 = v[:, :, 0, :], v[:, :, 1, :]
                cs = self.cos[:, t, :].unsqueeze(1).broadcast_to([128, 4, 64])
                sn = self.sin[:, t, :].unsqueeze(1).broadcast_to([128, 4, 64])
                tmp = rot_tmp[t % 2]
                ro = rot_o[t % 2]
                tt = nc.vector.tensor_tensor
                k.op(k.dve, lambda: tt(out=rot_a[:], in0=x1, in1=cs, op=ALU.mult), R=[ps, self.cos], W=[rot_a])
                k.op(k.dve, lambda: tt(out=rot_b[:], in0=x2, in1=sn, op=ALU.mult), R=[ps, self.sin], W=[rot_b])
                k.op(k.dve, lambda: tt(out=tmp[:, :, 0, :], in0=rot_a[:], in1=rot_b[:], op=ALU.subtract), R=[rot_a, rot_b], W=[tmp])
                k.op(k.dve, lambda: tt(out=rot_a[:], in0=x1, in1=sn, op=ALU.mult), R=[ps, self.sin], W=[rot_a])
                k.op(k.dve, lambda: tt(out=rot_b[:], in0=x2, in1=cs, op=ALU.mult), R=[ps, self.cos], W=[rot_b])
                k.op(k.dve, lambda: tt(out=tmp[:, :, 1, :], in0=rot_a[:], in1=rot_b[:], op=ALU.add), R=[rot_a, rot_b], W=[tmp])
                dcb = dec[:, t, :].unsqueeze(2).broadcast_to([128, 4, 128])
                k.op(k.dve, lambda: tt(out=ro[:], in0=tmp[:].rearrange("p h two d -> p h (two d)"), in1=dcb, op=ALU.mult),
                     R=[tmp, dec], W=[ro])
                if t >= 1:
                    emit_tr(t - 1, dstT)
            emit_tr(NT - 1, dstT)
        wb = load_w(2560)
        for t in range(NT):
            ps = proj_tm(wb, t, 2 + t % 2)
            k.op(k.act, lambda: nc.scalar.copy(out=r_v[:, t, :], in_=ps[:]), R=[ps], W=[r_v])
        if self.cfg.get('mx_stop', 99) <= 2:
            return
        self.new_phase()
        R4.reset()
        ATs = [R4.alloc([512], BF16, "AT%d" % i) for i in range(3)]
        sq = [R4.alloc([4, 128], F32, "sq%d" % i) for i in range(1)]
        cen = R4.alloc([4, 128], F32, "cen")
        st = {nm: R4.alloc([4], F32, nm) for nm in ("s1", "ssq", "mean", "m2", "var", "lnv", "rstd")}
        eps4 = R4.alloc([1], F32, "eps4")
        k.op(k.dve, lambda: nc.vector.memset(eps4[:], EPS), W=[eps4])
        free_slots = [2, 1, 0]

        def ret_chain(h, tg):
            def gen():
                i = free_slots.pop()
                ST, OUT, AT = self.ps[i], self.ps[3 + i], ATs[i]
                k.op(k.dve, lambda: nc.vector.memset(OUT[:], 0.0), W=[OUT])
                yield
                for c in range(4 * tg + 3, -1, -1):
                    t0 = max(512 * tg, 128 * c)
                    w = 512 * (tg + 1) - t0
                    off = t0 - 512 * tg
                    k.op(k.pe, lambda: nc.tensor.matmul(ST[:, 0:w], lhsT=rkT[:, h, c * 128:(c + 1) * 128], rhs=rqT[:, h, t0:t0 + w],
                                                        start=True, stop=True), R=[rkT, rqT], W=[ST])
                    yield
                    k.op(k.act, lambda: nc.scalar.copy(out=AT[:, 0:w], in_=ST[:, 0:w]), R=[ST], W=[AT])
                    if c >= 4 * tg:
                        k.op(k.pool, lambda: nc.gpsimd.affine_select(out=AT[:, 0:128], in_=AT[:, 0:128], pattern=[[1, 128]],
                                                                     compare_op=ALU.is_ge, fill=0.0, base=0, channel_multiplier=-1),
                             R=[AT], W=[AT])
                    yield
                    nq = w // 128
                    for j in range(nq):
                        qbl = off // 128 + j
                        k.op(k.pe, lambda j=j, qbl=qbl: nc.tensor.matmul(OUT[:, qbl * 128:(qbl + 1) * 128], lhsT=AT[:, j * 128:(j + 1) * 128],
                                                                         rhs=r_v[:, c, h * 128:(h + 1) * 128], start=False, stop=False,
                                                                         skip_group_check=True),
                             R=[AT, r_v], W=[OUT], sig=(j == nq - 1))
                    yield
                o3 = OUT[:].rearrange("p (q d) -> p q d", q=4)
                k.op(k.dve, lambda: nc.vector.tensor_reduce(out=st["s1"][:], in_=o3, axis=AX.X, op=ALU.add), R=[OUT], W=[st["s1"]])
                k.op(k.act, lambda: nc.scalar.activation(out=sq[0][:], in_=o3, func=AF.Square), R=[OUT], W=[sq[0]])
                k.op(k.dve, lambda: nc.vector.tensor_reduce(out=st["ssq"][:], in_=sq[0][:], axis=AX.X, op=ALU.add), R=[sq[0]], W=[st["ssq"]])
                k.op(k.dve, lambda: nc.vector.tensor_scalar(out=st["mean"][:], in0=st["s1"][:], scalar1=1.0 / 128, scalar2=None, op0=ALU.mult),
                     R=[st["s1"]], W=[st["mean"]])
                k.op(k.dve, lambda: nc.vector.tensor_tensor(out=st["m2"][:], in0=st["mean"][:], in1=st["mean"][:], op=ALU.mult),
                     R=[st["mean"]], W=[st["m2"]])
                k.op(k.dve, lambda: nc.vector.scalar_tensor_tensor(out=st["var"][:], in0=st["ssq"][:], scalar=1.0 / 128, in1=st["m2"][:],
                                                                   op0=ALU.mult, op1=ALU.subtract),
                     R=[st["ssq"], st["m2"]], W=[st["var"]])
                k.op(k.act, lambda: nc.scalar.activation(out=st["lnv"][:], in_=st["var"][:], func=AF.Ln, bias=eps4[:]),
                     R=[st["var"], eps4], W=[st["lnv"]])
                k.op(k.act, lambda: nc.scalar.activation(out=st["rstd"][:], in_=st["lnv"][:], func=AF.Exp, scale=-0.5),
                     R=[st["lnv"]], W=[st["rstd"]])
                k.op(k.dve, lambda: nc.vector.tensor_tensor(out=cen[:], in0=o3, in1=st["mean"][:].unsqueeze(2).broadcast_to([128, 4, 128]),
                                                            op=ALU.subtract), R=[OUT, st["mean"]], W=[cen])
                k.op(k.dve, lambda: nc.vector.tensor_tensor(out=ret_n[:, 4 * tg:4 * tg + 4, h * 128:(h + 1) * 128], in0=cen[:],
                                                            in1=st["rstd"][:].unsqueeze(2).broadcast_to([128, 4, 128]), op=ALU.mult),
                     R=[cen, st["rstd"]], W=[ret_n])
                free_slots.append(i)
                yield
            return gen

        run_chains([ret_chain(h, tg) for h in range(4) for tg in range(4)], 3)
        if self.cfg.get('mx_stop', 99) <= 3:
            return
        self.new_phase()
        R4.reset()
        sg = [R4.alloc([512], F32, "sg%d" % i) for i in range(2)]
        wb = load_w(3072)
        for t in range(NT):
            ps = proj_tm(wb, t, t % 2)
            k.op(k.act, lambda: nc.scalar.activation(out=sg[t % 2][:], in_=ps[:], func=AF.Silu), R=[ps], W=[sg[t % 2]])
            k.op(k.dve, lambda: nc.vector.tensor_tensor(out=ret_n[:, t, :], in0=ret_n[:, t, :], in1=sg[t % 2][:], op=ALU.mult),
                 R=[ret_n, sg[t % 2]], W=[ret_n])
        if self.cfg.get('mx_stop', 99) <= 4:
            return
        self.new_phase()
        R1.reset()
        qT2 = R1.alloc([4, S], BF16, "qT2")
        kT2 = R1.alloc([4, S], BF16, "kT2")
        v_sb = R1.alloc([NT, 512], BF16, "v_sb")
        nev = [0]
        for col0, dstT in ((0, qT2), (512, kT2)):
            wb = load_w(col0)
            for p in range(4):
                for g in range(4):
                    ps = self.ps[nev[0] % 4]
                    for c in range(DC):
                        k.op(k.pe, lambda c=c: nc.tensor.matmul(ps[:], lhsT=wb[:, c, p * 128:(p + 1) * 128], rhs=hT[g][:, c, :],
                                                                start=(c == 0), stop=(c == DC - 1)),
                             R=[hT[g], wb], W=[ps], sig=(c == DC - 1))
                    if nev[0] % 2 == 0:
                        k.op(k.act, lambda: nc.scalar.copy(out=dstT[:, p, g * 512:(g + 1) * 512], in_=ps[:]), R=[ps], W=[dstT])
                    else:
                        k.op(k.dve, lambda: nc.vector.tensor_copy(out=dstT[:, p, g * 512:(g + 1) * 512], in_=ps[:]), R=[ps], W=[dstT])
                    nev[0] += 1
        wb = load_w(1024)
        for t in range(NT):
            ps = proj_tm(wb, t, 4 + t % 2)
            k.op(k.act, lambda: nc.scalar.copy(out=v_sb[:, t, :], in_=ps[:]), R=[ps], W=[v_sb])
        if self.cfg.get('mx_stop', 99) <= 5:
            return
        self.new_phase()
        R3.reset()
        R0.reset()
        R4.reset()
        sb_n = R3.alloc([NT, 512], BF16, "sb_n")
        eT = [[R0.alloc([512], F32, "eT") for _ in range(2)] for _ in range(3)]
        PT = [[R0.alloc([512], BF16, "PT") for _ in range(2)] for _ in range(3)]
        E2 = [[R0.alloc([512], F32, "E2") for _ in range(1)] for _ in range(3)]
        AT2 = [[R0.alloc([512], BF16, "AT2") for _ in range(1)] for _ in range(3)]
        sq2 = [R4.alloc([4, 64], F32, "sq2") for _ in range(3)]
        st2 = [{nm: R4.alloc([4], F32, nm) for nm in ("ssq", "lnv", "rstd")} for _ in range(3)]
        eps5 = R4.alloc([1], F32, "eps5")
        k.op(k.dve, lambda: nc.vector.memset(eps5[:], EPS), W=[eps5])
        free_slots = [2, 1, 0]
        out_views = [(self.ps[6], 0), (self.ps[6], 256), (self.ps[7], 0)]

        def sb_chain(h, tg):
            def gen():
                i = free_slots.pop()
                ZTs, ACC = [self.ps[i], self.ps[i]], self.ps[3 + i]
                OUT, oo = out_views[i]
                pr, pb = divmod(h, 2)
                pb *= 64
                units = list(range(4 * tg + 3, -1, -1))

                def geom(c):
                    t0 = max(512 * tg, 128 * c)
                    return t0, 512 * (tg + 1) - t0, t0 - 512 * tg

                def front(n):
                    c = units[n]
                    t0, w, off = geom(c)
                    ZT, e_, P_ = ZTs[n % 2], eT[i][n % 2], PT[i][n % 2]
                    k.op(k.pe, lambda: nc.tensor.matmul(ZT[:, 0:w], lhsT=kT2[pb:pb + 64, pr, c * 128:(c + 1) * 128],
                                                        rhs=qT2[pb:pb + 64, pr, t0:t0 + w], start=True, stop=True),
                         R=[kT2, qT2], W=[ZT])
                    yield
                    k.op(k.act, lambda: nc.scalar.activation(out=e_[:, 0:w], in_=ZT[:, 0:w], func=AF.Exp, scale=0.125), R=[ZT], W=[e_])
                    if c >= 4 * tg:
                        k.op(k.pool, lambda: nc.gpsimd.affine_select(out=e_[:, 0:128], in_=e_[:, 0:128], pattern=[[1, 128]],
                                                                     compare_op=ALU.is_gt, fill=0.0, base=0, channel_multiplier=-1),
                             R=[e_], W=[e_])
                    yield
                    k.op(k.act, lambda: nc.scalar.activation(out=P_[:, 0:w], in_=e_[:, 0:w], func=AF.Ln, bias=1.0), R=[e_], W=[P_])
                    yield

                def back(n):
                    c = units[n]
                    t0, w, off = geom(c)
                    e_, P_, E2_, A_ = eT[i][n % 2], PT[i][n % 2], E2[i][0], AT2[i][0]
                    k.op(k.pe, lambda: nc.tensor.matmul(ACC[:, off:off + w], lhsT=self.tri_incl[:], rhs=P_[:, 0:w], start=False, stop=False,
                                                        skip_group_check=True), R=[self.tri_incl, P_], W=[ACC])
                    yield
                    k.op(k.act, lambda: nc.scalar.activation(out=E2_[:, 0:w], in_=ACC[:, off:off + w], func=AF.Exp, scale=-1.0),
                         R=[ACC], W=[E2_])
                    yield
                    k.op(k.pe, lambda: nc.tensor.matmul(ACC[:, off:off + w], lhsT=self.tri_low[:], rhs=P_[:, 0:w], start=False, stop=False,
                                                        skip_group_check=True), R=[self.tri_low, P_], W=[ACC])
                    k.op(k.dve, lambda: nc.vector.tensor_tensor(out=A_[:, 0:w], in0=e_[:, 0:w], in1=E2_[:, 0:w], op=ALU.mult),
                         R=[e_, E2_], W=[A_])
                    yield
                    nq = w // 128
                    for j in range(nq):
                        qbl = off // 128 + j
                        k.op(k.pe, lambda j=j, qbl=qbl: nc.tensor.matmul(OUT[:, oo + qbl * 64:oo + (qbl + 1) * 64], lhsT=A_[:, j * 128:(j + 1) * 128],
                                                                         rhs=v_sb[:, c, h * 64:(h + 1) * 64], start=False, stop=False,
                                                                         skip_group_check=True),
                             R=[A_, v_sb], W=[OUT], sig=(j == nq - 1))
                    yield

                k.op(k.dve, lambda: nc.vector.memset(ACC[:], 0.0), W=[ACC])
                k.op(k.dve, lambda: nc.vector.memset(OUT[:, oo:oo + 256], 0.0), W=[OUT])
                yield
                yield from front(0)
                for n in range(len(units)):
                    if n + 1 < len(units):
                        yield from front(n + 1)
                    yield from back(n)
                o3 = OUT[:, oo:oo + 256].rearrange("p (q d) -> p q d", q=4)
                sq_, s_ = sq2[i], st2[i]
                k.op(k.act, lambda: nc.scalar.activation(out=sq_[:], in_=o3, func=AF.Square), R=[OUT], W=[sq_])
                k.op(k.dve, lambda: nc.vector.tensor_reduce(out=s_["ssq"][:], in_=sq_[:], axis=AX.X, op=ALU.add), R=[sq_], W=[s_["ssq"]])
                k.op(k.act, lambda: nc.scalar.activation(out=s_["lnv"][:], in_=s_["ssq"][:], func=AF.Ln, scale=1.0 / 64, bias=eps5[:]),
                     R=[s_["ssq"], eps5], W=[s_["lnv"]])
                k.op(k.act, lambda: nc.scalar.activation(out=s_["rstd"][:], in_=s_["lnv"][:], func=AF.Exp, scale=-0.5),
                     R=[s_["lnv"]], W=[s_["rstd"]])
                k.op(k.dve, lambda: nc.vector.tensor_tensor(out=sb_n[:, 4 * tg:4 * tg + 4, h * 64:(h + 1) * 64], in0=o3,
                                                            in1=s_["rstd"][:].unsqueeze(2).broadcast_to([128, 4, 64]), op=ALU.mult),
                     R=[OUT, s_["rstd"]], W=[sb_n])
                free_slots.append(i)
                yield
            return gen

        run_chains([sb_chain(h, tg) for h in range(8) for tg in range(4)], 3)
        if self.cfg.get('mx_stop', 99) <= 6:
            return
        self.new_phase()
        R1.reset()
        R4.reset()
        if self.cfg.get("dbg"):
            k.dma(k.sp, self.dbg_ret, ret_n[:], R=[ret_n])
            k.dma(k.sp, self.dbg_sb, sb_n[:], R=[sb_n])
        wo = R1.alloc([DC, D], BF16, "wo")
        k.dma(k.sp, wo[:], self.w_out_b[l][:].rearrange("(c p) n -> p c n", p=128), R=[self.w_out_b[l]], W=[wo])
        mixT = [R4.alloc([DC, 128], BF16, "mixT%d" % i) for i in range(2)]
        gc = self.gcols[:, gb + 32:gb + 40].unsqueeze(2).broadcast_to([128, DC, 128])

        def emit_T(t):
            pT = self.ps[t % 2]
            for c in range(DC):
                src_t, src = (sb_n, sb_n[:, t, c * 128:(c + 1) * 128]) if c < 4 else (ret_n, ret_n[:, t, (c - 4) * 128:(c - 3) * 128])
                k.op(k.pe, lambda c=c, src=src: nc.tensor.transpose(out=pT.bf[:, c * 128:(c + 1) * 128], in_=src, identity=self.ident_b[:]),
                     R=[src_t, self.ident_b], W=[pT], sig=(c == DC - 1))
            mt = mixT[t % 2]
            k.op(k.dve, lambda: nc.vector.tensor_tensor(out=mt[:], in0=pT.bf.rearrange("p (c t) -> p c t", c=DC), in1=gc, op=ALU.mult),
                 R=[pT, self.gcols], W=[mt])

        emit_T(0)
        for t in range(NT):
            if t + 1 < NT:
                emit_T(t + 1)
            mt = mixT[t % 2]
            for half in range(2):
                ps = self.ps[2 + 2 * (t % 2) + half]
                for c in range(DC):
                    k.op(k.pe, lambda c=c: nc.tensor.matmul(ps[:], lhsT=mt[:, c, :], rhs=wo[:, c, half * 512:(half + 1) * 512],
                                                            start=(c == 0), stop=(c == DC - 1)),
                         R=[mt, wo], W=[ps], sig=(c == DC - 1))
                xa = self.x[t][:, half * 512:(half + 1) * 512]
                k.op(k.dve, lambda: nc.vector.tensor_tensor(out=xa, in0=xa, in1=ps[:], op=ALU.add), R=[self.x[t], ps], W=[self.x[t]])

    def xattn(self, l, b):
        k, nc = self.k, self.nc
        gb = 40 * l
        R0 = self.region(0, 32)
        R1 = self.region(32, 80)
        R2 = self.region(80, 96)
        R3 = self.region(96, 112)
        R4 = self.region(112, 122)
        self.new_phase()
        hT = [R0.alloc([DC, 512], BF16, "hT%d" % g) for g in range(4)]
        qT = R1.alloc([DC, S], BF16, "qT")
        memT = R1.alloc([DC, NM], BF16, "memT")
        kT = R1.alloc([DC, NM], BF16, "kT")
        vv = R1.alloc([2, D], BF16, "vv")
        memx = [R2.alloc([D], F32, "memx%d" % i) for i in range(2)]
        self.eps_t = R4.alloc([1], F32, "eps")
        k.op(k.dve, lambda: nc.vector.memset(self.eps_t[:], EPS), W=[self.eps_t])
        wks = [(R4.alloc([D], BF16, "junk"), R4.alloc([1], F32, "ss"), R4.alloc([1], F32, "lnv"), R4.alloc([1], F32, "rstd"),
                R4.alloc([D], BF16, "hn")) for _ in range(2)]
        for i in range(2):
            k.dma(k.sp, memx[i][:], self.mem_d[b, i * 128:(i + 1) * 128, :], W=[memx[i]])
        for i in range(2):
            self.norm_T(memx[i], memx[i][:], gb + 16, memT, memT[:, :, i * 128:(i + 1) * 128], wks[i], i)
        for t in range(NT):
            g, tl = divmod(t, 4)
            self.norm_T(self.x[t], self.x[t][:], gb + 8, hT[g], hT[g][:, :, tl * 128:(tl + 1) * 128], wks[t % 2], t % 2)
        wbuf = [R3.alloc([DC, 512], BF16, "wbuf%d" % i) for i in range(2)]
        nwb = [0]

        def load_w(src_tile, src_ap):
            wb = wbuf[nwb[0] % 2]
            nwb[0] += 1
            k.dma(k.sp, wb[:], src_ap.rearrange("(c p) n -> p c n", p=128), R=[src_tile], W=[wb])
            return wb

        nev = [0]

        def evac(out_ap, out_tile, ps, in_ap):
            if nev[0] % 2 == 0:
                k.op(k.act, lambda: nc.scalar.copy(out=out_ap, in_=in_ap), R=[ps], W=[out_tile])
            else:
                k.op(k.dve, lambda: nc.vector.tensor_copy(out=out_ap, in_=in_ap), R=[ps], W=[out_tile])
            nev[0] += 1

        for half in range(2):
            wb = load_w(self.xa_wkv_b[l], self.xa_wkv_b[l][:, half * 512:(half + 1) * 512])
            for j in range(4):
                ps = self.ps[2 + nev[0] % 4]
                for c in range(DC):
                    k.op(k.pe, lambda c=c: nc.tensor.matmul(ps[:, 0:NM], lhsT=wb[:, c, j * 128:(j + 1) * 128], rhs=memT[:, c, :],
                                                            start=(c == 0), stop=(c == DC - 1)), R=[wb, memT], W=[ps], sig=(c == DC - 1))
                evac(kT[:, half * 4 + j, :], kT, ps, ps[:, 0:NM])
        for half in range(2):
            wb = load_w(self.xa_wkv_b[l], self.xa_wkv_b[l][:, D + half * 512:D + (half + 1) * 512])
            for mt in range(2):
                ps = self.ps[2 + nev[0] % 4]
                for c in range(DC):
                    k.op(k.pe, lambda c=c: nc.tensor.matmul(ps[:], lhsT=memT[:, c, mt * 128:(mt + 1) * 128], rhs=wb[:, c, :],
                                                            start=(c == 0), stop=(c == DC - 1)), R=[wb, memT], W=[ps], sig=(c == DC - 1))
                evac(vv[:, mt, half * 512:(half + 1) * 512], vv, ps, ps[:])
        for half in range(2):
            wb = load_w(self.xa_wq_b[l], self.xa_wq_b[l][:, half * 512:(half + 1) * 512])
            for j in range(4):
                for g in range(4):
                    ps = self.ps[2 + nev[0] % 4]
                    for c in range(DC):
                        k.op(k.pe, lambda c=c: nc.tensor.matmul(ps[:], lhsT=wb[:, c, j * 128:(j + 1) * 128], rhs=hT[g][:, c, :],
                                                                start=(c == 0), stop=(c == DC - 1)), R=[wb, hT[g]], W=[ps], sig=(c == DC - 1))
                    evac(qT[:, half * 4 + j, g * 512:(g + 1) * 512], qT, ps, ps[:])
        self.new_phase()
        R0.reset()
        R4.reset()
        R2.reset()
        oT = [R0.alloc([DC, 512], BF16, "oT%d" % g) for g in range(4)]
        pT = [[R4.alloc([512], BF16, "pT") for _ in range(2)] for _ in range(2)]
        rec = [R4.alloc([512], F32, "rec") for _ in range(2)]
        wo = R2.alloc([DC, D], BF16, "wo")
        k.dma(k.sp, wo[:], self.xa_wo_b[l][:].rearrange("(c p) n -> p c n", p=128), R=[self.xa_wo_b[l]], W=[wo])
        it = 0
        for g in range(4):
            for h in range(4):
                pp = pT[it % 2]
                for mt in range(2):
                    ps = self.ps[(it % 2) * 2 + mt]
                    for kc in range(2):
                        k.op(k.pe, lambda kc=kc: nc.tensor.matmul(ps[:], lhsT=kT[:, 2 * h + kc, mt * 128:(mt + 1) * 128],
                                                                  rhs=qT[:, 2 * h + kc, g * 512:(g + 1) * 512], start=(kc == 0), stop=(kc == 1)),
                             R=[kT, qT], W=[ps], sig=(kc == 1))
                    k.op(k.act, lambda: nc.scalar.activation(out=pp[mt][:], in_=ps[:], func=AF.Exp, scale=1.0 / 16), R=[ps], W=[pp[mt]])
                sm = self.ps[4]
                for mt in range(2):
                    k.op(k.pe, lambda mt=mt: nc.tensor.matmul(sm[:], lhsT=self.ones_b[:], rhs=pp[mt][:], start=(mt == 0), stop=(mt == 1)),
                         R=[self.ones_b, pp[mt]], W=[sm], sig=(mt == 1))
                rc = rec[it % 2]
                k.op(k.dve, lambda: nc.vector.reciprocal(out=rc[:], in_=sm[:]), R=[sm], W=[rc])
                for dvc in range(2):
                    po = self.ps[5 + dvc]
                    for mt in range(2):
                        k.op(k.pe, lambda mt=mt: nc.tensor.matmul(po[:], lhsT=vv[:, mt, h * 256 + dvc * 128:h * 256 + (dvc + 1) * 128], rhs=pp[mt][:],
                                                                  start=(mt == 0), stop=(mt == 1)), R=[vv, pp[mt]], W=[po], sig=(mt == 1))
                    k.op(k.dve, lambda: nc.vector.tensor_tensor(out=oT[g][:, 2 * h + dvc, :], in0=po[:], in1=rc[:], op=ALU.mult),
                         R=[po, rc], W=[oT[g]])
                it += 1
        for t in range(NT):
            g, tl = divmod(t, 4)
            for half in range(2):
                ps = self.ps[(t % 2) * 2 + half]
                for c in range(DC):
                    k.op(k.pe, lambda c=c: nc.tensor.matmul(ps[:], lhsT=oT[g][:, c, tl * 128:(tl + 1) * 128], rhs=wo[:, c, half * 512:(half + 1) * 512],
                                                            start=(c == 0), stop=(c == DC - 1)), R=[oT[g], wo], W=[ps], sig=(c == DC - 1))
                xa = self.x[t][:, half * 512:(half + 1) * 512]
                k.op(k.dve, lambda: nc.vector.tensor_tensor(out=xa, in0=xa, in1=ps[:], op=ALU.add), R=[self.x[t], ps], W=[self.x[t]])


    def peer(self, l):
        k, nc = self.k, self.nc
        gb = 40 * l
        RW = self.region(0, 64)
        RS = self.region(64, 96)
        RH = self.region(96, 104)
        RP = self.region(104, 136)
        self.new_phase()
        tt = nc.vector.tensor_tensor
        W_sb = RW.alloc([TB, 128], BF16, "W_sb")
        NSB = 4
        sbuf = [RS.alloc([4096], BF16, "sbuf%d" % i) for i in range(NSB)]
        hTbs = [RH.alloc([DC, TB], BF16, "hTb%d" % i) for i in range(2)]
        nsb = [0]

        def next_sbuf():
            b_ = sbuf[nsb[0] % NSB]
            nsb[0] += 1
            return b_

        qTb = RP.alloc([16, 128], BF16, "qTb")
        self.eps_t = RP.alloc([1], F32, "eps")
        k.op(k.dve, lambda: nc.vector.memset(self.eps_t[:], EPS), W=[self.eps_t])
        hn_ = RP.alloc([D], BF16, "hn")
        wk = (hn_, RP.alloc([1], F32, "ss"), RP.alloc([1], F32, "lnv"), RP.alloc([1], F32, "rstd"), hn_)
        tops = RP.alloc([16, 16], F32, "tops")
        tidx = RP.alloc([16, 16], U32, "tidx")
        tidxf = RP.alloc([16, 16], F32, "tidxf")
        cand = RP.alloc([16, 16], F32, "cand")
        cand2 = RP.alloc([256], F32, "cand2")
        s2 = cand2
        best = RP.alloc([8, 16], F32, "best")
        pos = RP.alloc([8, 16], U32, "pos")
        k1u = RP.alloc([8, 16], U32, "k1u")
        k2u = RP.alloc([8, 16], U32, "k2u")
        k1f = RP.alloc([8, 16], F32, "k1f")
        k2f = RP.alloc([8, 16], F32, "k2f")
        eq = RP.alloc([2, 16, 16], BF16, "eq")
        prod = RP.alloc([2, 16, 16], BF16, "prod")
        ex = RP.alloc([8, 16], F32, "ex")
        ssum = RP.alloc([8], F32, "ssum")
        g_tm = RP.alloc([8, 16], F32, "g_tm")
        I_tm = RP.alloc([8, 16], F32, "I_tm")
        J_tm = RP.alloc([8, 16], F32, "J_tm")
        Ism = RP.alloc([TB], F32, "Ism")
        Jsm = RP.alloc([TB], F32, "Jsm")
        gsm = RP.alloc([TB], F32, "gsm")
        TS = 4
        OJ = [RP.alloc([TS, 128], BF16, "OJ") for _ in range(2)]
        OI = [RP.alloc([TS, 128], BF16, "OI") for _ in range(2)]
        NB = 3
        ga = [RP.alloc([TB], BF16, "ga") for _ in range(2)]
        keysT = RP.alloc([16, 128], BF16, "keysT")
        k.dma(k.sp, keysT[:], self.keysT_b[l][:].rearrange("p (g n) -> p g n", g=16), R=[self.keysT_b[l]], W=[keysT])
        G = [RP.alloc([TB], BF16, "G") for _ in range(NB)]
        OUT = [[self.ps[0], self.ps[1]], [self.ps[2], self.ps[3]]]
        PA = [self.ps[4], self.ps[5]]
        PB = [self.ps[6], self.ps[7]]

        def part1(blk):
            hTb = hTbs[blk % 2]

            def emit_tr(ti_):
                pt = PB[0]
                for j, src in enumerate((I_tm, J_tm, g_tm)):
                    k.op(k.pe, lambda j=j, src=src: nc.tensor.transpose(out=pt[:, j * 128:(j + 1) * 128], in_=src[:].rearrange("p a b -> p (a b)"),
                                                                        identity=self.ident_f[:]), R=[src, self.ident_f], W=[pt], sig=(j == 2))
                for j, dst in enumerate((Ism, Jsm, gsm)):
                    k.op(k.act, lambda j=j, dst=dst: nc.scalar.copy(out=dst[:, ti_ * 128:(ti_ + 1) * 128], in_=pt[:, j * 128:(j + 1) * 128]), R=[pt], W=[dst])

            for ti in range(2):
                t = blk * 2 + ti
                self.norm_act(self.x[t], self.x[t][:], wk)
                for _ in range(4):
                    yield
                self.norm_tr(gb + 24, hTb, hTb[:, :, ti * 128:(ti + 1) * 128], wk, 6)
                yield
            kb = keysT
            kv = keysT[:]
            for ti in range(2):
                for q4 in range(4):
                    wb = next_sbuf()
                    wv = wb[:].rearrange("p (c n) -> p c n", c=DC)
                    k.dma(k.sp, wv, self.peer_wq_b[l][:, q4 * 512:(q4 + 1) * 512].rearrange("(c p) n -> p c n", p=128), R=[self.peer_wq_b[l]], W=[wb])
                    ps = PB[1]
                    for j in range(4):
                        for c in range(DC):
                            k.op(k.pe, lambda c=c, j=j: nc.tensor.matmul(ps[:, j * 128:(j + 1) * 128], lhsT=wv[:, c, j * 128:(j + 1) * 128],
                                                                         rhs=hTb[:, c, ti * 128:(ti + 1) * 128],
                                                                         start=(c == 0), stop=(c == DC - 1)), R=[wb, hTb], W=[ps], sig=(c == DC - 1))
                        yield
                    k.op(k.act, lambda: nc.scalar.copy(out=qTb[:, q4 * 4:q4 * 4 + 4, :], in_=ps[:].rearrange("p (j t) -> p j t", j=4)), R=[ps], W=[qTb])
                    yield
                if ti == 1:
                    emit_tr(0)
                for g4 in range(4):
                    ps = PB[g4 % 2]
                    for gi in range(4):
                        gq = g4 * 4 + gi
                        k.op(k.pe, lambda gq=gq, gi=gi: nc.tensor.matmul(ps[:, gi * 128:(gi + 1) * 128], lhsT=qTb[:, gq, :],
                                                                         rhs=kv[:, gq, :], start=True, stop=True), R=[qTb, kb], W=[ps], sig=(gi == 3))
                    yield
                    for gi in range(4):
                        gq = g4 * 4 + gi
                        sc = ps[:, gi * 128:(gi + 1) * 128]
                        k.op(k.dve, lambda: nc.vector.max(out=tops[:, gq, 0:8], in_=sc), R=[ps], W=[tops])
                        k.op(k.dve, lambda: nc.vector.max_index(out=tidx[:, gq, 0:8], in_max=tops[:, gq, 0:8], in_values=sc), R=[ps, tops], W=[tidx])
                        k.op(k.dve, lambda: nc.vector.match_replace(out=s2[:, 0:128], in_to_replace=tops[:, gq, 0:8], in_values=sc, imm_value=-1e30),
                             R=[ps, tops], W=[s2])
                        yield
                        k.op(k.dve, lambda: nc.vector.max(out=tops[:, gq, 8:16], in_=s2[:, 0:128]), R=[s2], W=[tops])
                        k.op(k.dve, lambda: nc.vector.max_index(out=tidx[:, gq, 8:16], in_max=tops[:, gq, 8:16], in_values=s2[:, 0:128]), R=[s2, tops], W=[tidx])
                        yield
                k.op(k.dve, lambda: nc.vector.tensor_copy(out=tidxf[:], in_=tidx[:]), R=[tidx], W=[tidxf])
                for h in range(8):
                    k.op(k.dve, lambda: tt(out=cand[:], in0=tops[:, 2 * h, :].unsqueeze(2).broadcast_to([128, 16, 16]),
                                           in1=tops[:, 2 * h + 1, :].unsqueeze(1).broadcast_to([128, 16, 16]), op=ALU.add), R=[tops], W=[cand])
                    cf = cand[:].rearrange("p a b -> p (a b)")
                    k.op(k.dve, lambda: nc.vector.max(out=best[:, h, 0:8], in_=cf), R=[cand], W=[best])
                    k.op(k.dve, lambda: nc.vector.max_index(out=pos[:, h, 0:8], in_max=best[:, h, 0:8], in_values=cf), R=[cand, best], W=[pos])
                    yield
                    k.op(k.dve, lambda: nc.vector.match_replace(out=cand2[:], in_to_replace=best[:, h, 0:8], in_values=cf, imm_value=-1e30),
                         R=[cand, best], W=[cand2])
                    k.op(k.dve, lambda: nc.vector.max(out=best[:, h, 8:16], in_=cand2[:]), R=[cand2], W=[best])
                    k.op(k.dve, lambda: nc.vector.max_index(out=pos[:, h, 8:16], in_max=best[:, h, 8:16], in_values=cand2[:]), R=[cand2, best], W=[pos])
                    yield
                k.op(k.dve, lambda: tt(out=ex[:], in0=best[:], in1=best[:, :, 0:1].broadcast_to([128, 8, 16]), op=ALU.subtract), R=[best], W=[ex])
                k.op(k.act, lambda: nc.scalar.activation(out=ex[:], in_=ex[:], func=AF.Exp), R=[ex], W=[ex])
                k.op(k.dve, lambda: nc.vector.tensor_reduce(out=ssum[:], in_=ex[:], axis=AX.X, op=ALU.add), R=[ex], W=[ssum])
                k.op(k.dve, lambda: nc.vector.reciprocal(out=ssum[:], in_=ssum[:]), R=[ssum], W=[ssum])
                k.op(k.dve, lambda: tt(out=g_tm[:], in0=ex[:], in1=ssum[:].unsqueeze(2).broadcast_to([128, 8, 16]), op=ALU.mult), R=[ex, ssum], W=[g_tm])
                yield
                k.op(k.dve, lambda: nc.vector.tensor_single_scalar(out=k1u[:], in_=pos[:], scalar=4, op=ALU.logical_shift_right), R=[pos], W=[k1u])
                k.op(k.dve, lambda: nc.vector.tensor_single_scalar(out=k2u[:], in_=pos[:], scalar=15, op=ALU.bitwise_and), R=[pos], W=[k2u])
                k.op(k.dve, lambda: nc.vector.tensor_copy(out=k1f[:], in_=k1u[:]), R=[k1u], W=[k1f])
                k.op(k.dve, lambda: nc.vector.tensor_copy(out=k2f[:], in_=k2u[:]), R=[k2u], W=[k2f])
                yield
                io16 = self.iota_f[:, 0:16].unsqueeze(1).unsqueeze(1).broadcast_to([128, 2, 16, 16])
                tf4 = tidxf[:].rearrange("p (h two) k -> p h two k", two=2)
                for kf, half, dst in ((k1f, 0, I_tm), (k2f, 1, J_tm)):
                    for hh in range(4):
                        hs = slice(hh * 2, hh * 2 + 2)
                        k.op(k.dve, lambda: tt(out=eq[:], in0=kf[:, hs, :].unsqueeze(3).broadcast_to([128, 2, 16, 16]), in1=io16, op=ALU.is_equal),
                             R=[kf, self.iota_f], W=[eq])
                        k.op(k.dve, lambda: tt(out=prod[:], in0=eq[:], in1=tf4[:, hs, half, :].unsqueeze(2).broadcast_to([128, 2, 16, 16]), op=ALU.mult),
                             R=[eq, tidxf], W=[prod])
                        k.op(k.dve, lambda: nc.vector.tensor_reduce(out=dst[:, hs, :], in_=prod[:], axis=AX.X, op=ALU.add), R=[prod], W=[dst])
                        yield
                if ti == 1:
                    for _ in range(12):
                        yield
                    emit_tr(1)
                yield

        def part2(blk):
            iob = self.iota_f[:].unsqueeze(1).broadcast_to([128, TS, 128])
            banks = [self.ps[4], self.ps[5], self.ps[6], self.ps[7]]
            for sbi in range(TB // TS):
                t0 = sbi * TS
                oj, oi = OJ[sbi % 2], OI[sbi % 2]
                for q in range(TS):
                    tq = t0 + q
                    k.op(k.dve, lambda q=q, tq=tq: nc.vector.tensor_scalar(out=oj[:, q, :], in0=self.iota_b[:], scalar1=Jsm[:, tq:tq + 1], scalar2=None,
                                                                           op0=ALU.is_equal), R=[self.iota_b, Jsm], W=[oj], indep=True, sig=False)
                    k.op(k.dve, lambda q=q, tq=tq: nc.vector.tensor_scalar(out=oi[:, q, :], in0=self.iota_b[:], scalar1=Ism[:, tq:tq + 1],
                                                                           scalar2=gsm[:, tq:tq + 1], op0=ALU.is_equal, op1=ALU.mult),
                         R=[self.iota_b, Ism, gsm], W=[oi], indep=True, sig=(q == TS - 1))
                pw = banks[sbi % 4]
                for q in range(4):
                    k.op(k.pe, lambda q=q: nc.tensor.matmul(pw[:, q * 128:(q + 1) * 128], lhsT=oj[:, q, :], rhs=oi[:, q, :], start=True, stop=True),
                         R=[oj, oi], W=[pw], sig=(q == 3))
                k.op(k.act, lambda: nc.scalar.copy(out=W_sb[:, t0:t0 + 4, :], in_=pw[:].rearrange("p (t i) -> p t i", t=4)), R=[pw], W=[W_sb])

        def loop(blk):
            hTb = hTbs[blk % 2]
            for ts in range(2):
                for dh in range(2):
                    k.op(k.dve, lambda: nc.vector.memset(OUT[ts][dh][:], 0.0), W=[OUT[ts][dh]])
            sbfs = {}
            AHEAD = 2
            NCH = 2 * NUG
            for c in range(NCH + AHEAD):
                if c < NCH:
                    cg, cc = divmod(c, 2)
                    if cc == 0:
                        sbf = next_sbuf()
                        sbfs[cg] = sbf
                        k.dma(k.sp, sbf[:], self.UV_b[l][cg], R=[self.UV_b[l]], W=[sbf])
                    sbf = sbfs[cg]
                    utv = sbf[:, 0:2048].rearrange("p (c e) -> p c e", c=DC)
                    pa = PA[c % 2]
                    for dc in range(DC):
                        k.op(k.pe, lambda dc=dc: nc.tensor.matmul(pa[:, 0:TB], lhsT=utv[:, dc, cc * 128:(cc + 1) * 128], rhs=hTb[:, dc, :],
                                                                  start=(dc == 0), stop=(dc == DC - 1)), R=[sbf, hTb], W=[pa], sig=(dc == DC - 1))
                    ga_, G_ = ga[c % 2], G[c % NB]
                    k.op(k.act, lambda: nc.scalar.activation(out=ga_[:], in_=pa[:, 0:TB], func=AF.Gelu), R=[pa], W=[ga_])
                    k.op(k.pool, lambda: nc.gpsimd.tensor_tensor(out=G_[:], in0=ga_[:], in1=W_sb[:, :, c], op=ALU.mult), R=[ga_, W_sb], W=[G_])
                c2 = c - AHEAD
                if c2 >= 0:
                    cg2, cc2 = divmod(c2, 2)
                    sbf2 = sbfs[cg2]
                    vgv = sbf2[:, 2048:4096].rearrange("p (cc d) -> p cc d", cc=2)
                    G2 = G[c2 % NB]
                    for ts in range(2):
                        for dh in range(2):
                            k.op(k.pe, lambda ts=ts, dh=dh: nc.tensor.matmul(OUT[ts][dh][:], lhsT=G2[:, ts * 128:(ts + 1) * 128],
                                                                             rhs=vgv[:, cc2, dh * 512:(dh + 1) * 512], start=False, stop=False,
                                                                             skip_group_check=True),
                                 R=[G2, sbf2], W=[OUT[ts][dh]], sig=(ts == 1 and dh == 1))
                yield
            for ts in range(2):
                t = blk * 2 + ts
                for dh in range(2):
                    xa = self.x[t][:, dh * 512:(dh + 1) * 512]
                    k.op(k.dve, lambda: tt(out=xa, in0=xa, in1=OUT[ts][dh][:], op=ALU.add), R=[self.x[t], OUT[ts][dh]], W=[self.x[t]])
            yield

        def thin2(gen):
            i_ = 0
            for _ in gen:
                i_ += 1
                if i_ % 3 != 0:
                    yield
            yield

        nblk = S // TB
        for _ in part1(0):
            pass
        part2(0)
        for blk in range(nblk):
            chains = [lambda blk=blk: loop(blk)]
            if blk + 1 < nblk:
                chains.append(lambda blk=blk: thin2(part1(blk + 1)))
            run_chains(chains, 2)
            if blk + 1 < nblk:
                part2(blk + 1)

    def final(self, b):
        k, nc = self.k, self.nc
        self.new_phase()
        R = self.region(0, 32)
        fing = R.alloc([D], F32, "fing")
        k.dma(k.sp, fing[:], self.fing_d, W=[fing])
        eps = R.alloc([1], F32, "eps")
        k.op(k.dve, lambda: nc.vector.memset(eps[:], EPS), W=[eps])
        junk = R.alloc([D], BF16, "junk")
        ys = [R.alloc([D], F32, "y%d" % i) for i in range(2)]
        st = [(R.alloc([1], F32, "ss"), R.alloc([1], F32, "lnv"), R.alloc([1], F32, "rstd")) for _ in range(2)]
        for t in range(NT):
            ss, lnv, rstd = st[t % 2]
            y = ys[t % 2]
            xt = self.x[t]
            k.op(k.act, lambda: nc.scalar.activation(out=junk[:], in_=xt[:], func=AF.Square, accum_out=ss[:]), R=[xt], W=[junk, ss])
            k.op(k.act, lambda: nc.scalar.activation(out=lnv[:], in_=ss[:], func=AF.Ln, scale=1.0 / D, bias=eps[:]), R=[ss, eps], W=[lnv])
            k.op(k.act, lambda: nc.scalar.activation(out=rstd[:], in_=lnv[:], func=AF.Exp, scale=-0.5), R=[lnv], W=[rstd])
            k.op(k.dve, lambda: nc.vector.scalar_tensor_tensor(out=y[:], in0=xt[:], scalar=rstd[:], in1=fing[:], op0=ALU.mult, op1=ALU.mult),
                 R=[xt, rstd, fing], W=[y])
            k.dma(k.sp, self.out_d[b, t * 128:(t + 1) * 128, :], y[:], R=[y])


def build(cfg):
    nc = bass.Bass("TRN2", target_bir_lowering=False)
    stack = contextlib.ExitStack()
    with stack:
        b = Builder(nc, stack, cfg)
        b.new_phase()
        b.setup()
        for s in range(cfg["NSEQ"]):
            b.load_x(s)
            for l in range(cfg["L"]):
                if "mixer" in cfg["phases"]:
                    b.mixer(l)
                if "xattn" in cfg["phases"]:
                    b.xattn(l, s)
                if "peer" in cfg["phases"]:
                    b.peer(l)
            if "final" in cfg["phases"]:
                b.final(s)
            else:
                b.new_phase()
                b.store(s, b.out_d)
        b.k.barrier()
    return nc


def host_consts():
    pos = np.arange(S, dtype=np.float32)
    inv = (10000.0 ** (-np.arange(0, 128, 2, dtype=np.float32) / 128)).astype(np.float32)
    ang = pos[:, None] * inv[None, :]
    cos = np.cos(ang).astype(np.float32).reshape(NT, 128, 64).transpose(1, 0, 2)
    sin = np.sin(ang).astype(np.float32).reshape(NT, 128, 64).transpose(1, 0, 2)
    log_g = np.log1p(-np.power(2.0, -5.0 - np.arange(4, dtype=np.float64)))
    dq = np.exp(pos.astype(np.float64)[:, None] * log_g[None, :])
    dk = np.exp(-pos.astype(np.float64)[:, None] * log_g[None, :]) * (128.0 ** -0.5)
    dq = dq.astype(np.float32).reshape(NT, 128, 4).transpose(1, 0, 2)
    dk = dk.astype(np.float32).reshape(NT, 128, 4).transpose(1, 0, 2)
    return {"cos": np.ascontiguousarray(cos), "sin": np.ascontiguousarray(sin), "dq": np.ascontiguousarray(dq),
            "dk": np.ascontiguousarray(dk)}


def col_layout(v):
    return np.ascontiguousarray(np.asarray(v, np.float32).reshape(DC, 128).T)


def make_inputs(inp, cfg, ncores=1):
    nl = cfg["L"]
    nseq = cfg["NSEQ"]
    hc = host_consts()
    gcols = np.zeros((128, 40 * nl), np.float32)
    for l in range(nl):
        cat = np.concatenate([np.asarray(inp["sb_norm"][l], np.float32), np.asarray(inp["ret_norm"][l], np.float32)])
        for i, v in enumerate((inp["mix_norm"][l], inp["xa_norm"][l], inp["mem_norm"][l], inp["peer_norm"][l], cat)):
            gcols[:, 40 * l + 8 * i:40 * l + 8 * i + 8] = col_layout(v)
    shared = {
        "w_in": np.ascontiguousarray(inp["w_in"][:nl], np.float32),
        "w_out": np.ascontiguousarray(inp["w_out"][:nl], np.float32),
        "xa_wq": np.ascontiguousarray(inp["xa_wq"][:nl], np.float32),
        "xa_wkv": np.ascontiguousarray(inp["xa_wkv"][:nl], np.float32),
        "xa_wo": np.ascontiguousarray(inp["xa_wo"][:nl], np.float32),
        "peer_wq": np.ascontiguousarray(inp["peer_wq"][:nl], np.float32),
        "keysT": np.ascontiguousarray(np.transpose(np.asarray(inp["peer_keys"][:nl], np.float32), (0, 4, 1, 2, 3)).reshape(nl, 128, 2048)),
        "UT": np.ascontiguousarray(np.asarray(inp["peer_u"][:nl], np.float32).reshape(nl, NUG, UG, DC, 128).transpose(0, 1, 4, 3, 2)).reshape(nl, NUG, 128, DC * UG),
        "V": np.ascontiguousarray(inp["peer_v"][:nl], np.float32),
        "gcols": gcols,
        "fing": np.ascontiguousarray(np.broadcast_to(np.asarray(inp["final_norm"], np.float32)[None, :], (128, D))),
    }
    shared.update(hc)
    maps = []
    for c in range(ncores):
        m = dict(shared)
        m["x"] = np.ascontiguousarray(inp["x"][c * nseq:(c + 1) * nseq], np.float32)
        m["mem"] = np.ascontiguousarray(inp["mem"][c * nseq:(c + 1) * nseq], np.float32)
        maps.append(m)
    return maps


_CFG = {"L": L, "NSEQ": NSEQ, "phases": ["mixer", "xattn", "peer", "final"]}
_NC_CACHE = {}


def kernel(**inputs):
    ncores = 8
    cfg = _CFG
    if "nc" not in _NC_CACHE:
        _NC_CACHE["nc"] = build(cfg)
    nc = _NC_CACHE["nc"]
    in_maps = make_inputs(inputs, cfg, ncores=ncores)
    res = run_bass_kernel_spmd(nc, in_maps, core_ids=list(range(ncores)))
    outs = [np.asarray(r["out"]) for r in res.results]
    return np.concatenate(outs, axis=0).astype(np.float32)
```

```python
import contextlib
import numpy as np
import concourse.bass as bass
import concourse.mybir as mybir
from concourse.bass_utils import run_bass_kernel_spmd

F32 = mybir.dt.float32
BF16 = mybir.dt.bfloat16
U32 = mybir.dt.uint32
I32 = mybir.dt.int32
AF = mybir.ActivationFunctionType
ALU = mybir.AluOpType
AX = mybir.AxisListType

S = 2048
NT = 16
D = 1024
DC = 8
NM = 256
L = 2
NSEQ = 2
EPS = 1e-6
INC = 3584
NEXP = 16384
UG = 256
NUG = NEXP // UG
TB = 256


class Eng:
    def __init__(self, k, name, h, same_sync):
        self.name = name
        self.h = h
        self.sem = k.new_sem("e_" + name)
        self.count = 0
        self.seq = 0
        self.sigmap = {}
        self.pending = []
        self.seen = {}
        self.same_sync = same_sync


class DSem:
    def __init__(self, h):
        self.h = h
        self.count = 0


class Tile:
    __slots__ = ("ap", "name", "last_w", "readers", "dsem", "excl", "bf")

    def __init__(self, ap, name="", excl=False):
        self.ap = ap
        self.name = name
        self.last_w = None
        self.readers = {}
        self.dsem = None
        self.excl = excl
        self.bf = None

    def __getitem__(self, idx):
        return self.ap[idx]


class K:
    def __init__(self, nc, stack):
        self.nc = nc
        self.stack = stack
        self.pe = Eng(self, "pe", nc.tensor, False)
        self.act = Eng(self, "act", nc.scalar, True)
        self.dve = Eng(self, "dve", nc.vector, True)
        self.pool = Eng(self, "pool", nc.gpsimd, True)
        self.sp = Eng(self, "sp", nc.sync, False)
        self.engs = [self.pe, self.act, self.dve, self.pool, self.sp]
        self.dsems = []
        self.dsem_by_name = {}

    def new_sem(self, name):
        return self.stack.enter_context(self.nc.semaphore(name))

    def get_dsem(self, name):
        if name in self.dsem_by_name:
            return self.dsem_by_name[name]
        d = DSem(self.new_sem("d%d" % len(self.dsems)))
        self.dsems.append(d)
        self.dsem_by_name[name] = d
        return d

    def _resolve(self, eng, dep):
        if dep[0] == "e":
            src, seq = dep[1], dep[2]
            if src is eng and not eng.same_sync:
                return None
            cnt = src.sigmap.get(seq)
            if cnt is None:
                if src is eng:
                    return None
                raise RuntimeError("dependency on unsignaled instruction of %s" % src.name)
            return (src.sem, id(src), cnt)
        d, cnt = dep[1], dep[2]
        return (d.h, id(d), cnt)

    def _wait(self, eng, deps):
        best = {}
        for dep in deps:
            r = self._resolve(eng, dep)
            if r is None:
                continue
            sem, key, cnt = r
            if eng.seen.get(key, 0) >= cnt:
                continue
            if key not in best or best[key][1] < cnt:
                best[key] = (sem, cnt)
        for key, (sem, cnt) in best.items():
            eng.h.wait_ge(sem, cnt)
            eng.seen[key] = cnt

    @staticmethod
    def _gather(R, W):
        deps = []
        for t in R:
            if t.last_w is not None:
                deps.append(t.last_w)
        for t in W:
            if t.last_w is not None:
                deps.append(t.last_w)
            deps.extend(t.readers.values())
        return deps

    def op(self, eng, fn, R=(), W=(), sig=True, indep=False):
        ex = [t for t in R if t.excl]
        if ex:
            W = list(W) + [t for t in ex if t not in W]
        deps = self._gather(R, W)
        if indep:
            deps = [d for d in deps if not (d[0] == "e" and d[1] is eng)]
        self._wait(eng, deps)
        ins = fn()
        eng.seq += 1
        seq = eng.seq
        if sig:
            eng.count += 1
            ins.then_inc(eng.sem, 1)
            for s in eng.pending:
                eng.sigmap[s] = eng.count
            eng.pending = []
            eng.sigmap[seq] = eng.count
        else:
            eng.pending.append(seq)
        me = ("e", eng, seq)
        for t in R:
            t.readers[id(eng)] = me
        for t in W:
            t.last_w = me
            t.readers = {}
        return ins

    def dma(self, q, out, in_, R=(), W=(), semtile=None, **kw):
        self._wait(q, self._gather(R, W))
        if semtile is None:
            semtile = W[0] if len(W) else R[0]
        if semtile.dsem is None:
            semtile.dsem = self.get_dsem(semtile.name)
        d = semtile.dsem
        ins = q.h.dma_start(out=out, in_=in_, **kw)
        ins.then_inc(d.h, 16)
        d.count += 16
        me = ("d", d, d.count)
        for t in R:
            t.readers[id(d)] = me
        for t in W:
            t.last_w = me
            t.readers = {}
        return ins

    def barrier(self):
        for e in self.engs:
            if e.pending:
                raise RuntimeError("barrier with unsignaled pending instructions on " + e.name)
        for e in self.engs:
            for o in self.engs:
                if o.count == 0 or (o is e and not e.same_sync):
                    continue
                if e.seen.get(id(o), 0) < o.count:
                    e.h.wait_ge(o.sem, o.count)
                    e.seen[id(o)] = o.count
            for d in self.dsems:
                if d.count and e.seen.get(id(d), 0) < d.count:
                    e.h.wait_ge(d.h, d.count)
                    e.seen[id(d)] = d.count


def esize(dt):
    return 4 if dt in (F32, U32, I32) else 2


class Region:
    def __init__(self, arena, lo, hi):
        self.arena = arena
        self.lo = lo
        self.hi = hi
        self.off = lo

    def reset(self):
        self.off = self.lo

    def alloc(self, shape_free, dtype, name=""):
        n = int(np.prod(shape_free))
        nb = (n * esize(dtype) + 31) // 32 * 32
        if self.off + nb > self.hi:
            raise RuntimeError("region overflow %s: need %d at %d (hi %d)" % (name, nb, self.off, self.hi))
        a = self.arena[:, self.off // 2:(self.off + n * esize(dtype)) // 2]
        self.off += nb
        if dtype != BF16:
            a = a.bitcast(dtype)
        if len(shape_free) == 2:
            a = a.rearrange("p (a b) -> p a b", a=shape_free[0])
        elif len(shape_free) == 3:
            a = a.rearrange("p (a b c) -> p a b c", a=shape_free[0], b=shape_free[1])
        return Tile(a, name)


def run_chains(chains, width):
    chains = list(chains)
    active = []
    nxt = 0
    while nxt < len(chains) or active:
        while len(active) < width and nxt < len(chains):
            active.append(chains[nxt]())
            nxt += 1
        for g in list(active):
            try:
                next(g)
            except StopIteration:
                active.remove(g)


KB = 1024


class Builder:
    def __init__(self, nc, stack, cfg):
        self.nc = nc
        self.cfg = cfg
        self.k = K(nc, stack)
        k = self.k
        dt = nc.dram_tensor
        nl = cfg["L"]
        nseq = cfg["NSEQ"]
        self.nl, self.nseq = nl, nseq
        self.x_d = dt("x", [nseq, S, D], F32, kind="ExternalInput").ap()
        self.mem_d = dt("mem", [nseq, NM, D], F32, kind="ExternalInput").ap()
        self.out_d = dt("out", [nseq, S, D], F32, kind="ExternalOutput").ap()
        self.w_in_d = dt("w_in", [nl, D, INC], F32, kind="ExternalInput").ap()
        self.w_out_d = dt("w_out", [nl, D, D], F32, kind="ExternalInput").ap()
        self.gcols_d = dt("gcols", [128, 40 * nl], F32, kind="ExternalInput").ap()
        self.fing_d = dt("fing", [128, D], F32, kind="ExternalInput").ap()
        self.cos_d = dt("cos", [128, NT, 64], F32, kind="ExternalInput").ap()
        self.sin_d = dt("sin", [128, NT, 64], F32, kind="ExternalInput").ap()
        self.dq_d = dt("dq", [128, NT, 4], F32, kind="ExternalInput").ap()
        self.dk_d = dt("dk", [128, NT, 4], F32, kind="ExternalInput").ap()
        self.xa_wq_d = dt("xa_wq", [nl, D, D], F32, kind="ExternalInput").ap()
        self.xa_wkv_d = dt("xa_wkv", [nl, D, 2 * D], F32, kind="ExternalInput").ap()
        self.xa_wo_d = dt("xa_wo", [nl, D, D], F32, kind="ExternalInput").ap()
        self.xa_wq_b = [Tile(dt("xa_wq_b%d" % l_, [D, D], BF16, kind="Internal").ap(), "xa_wq_b%d" % l_) for l_ in range(nl)]
        self.xa_wkv_b = [Tile(dt("xa_wkv_b%d" % l_, [D, 2 * D], BF16, kind="Internal").ap(), "xa_wkv_b%d" % l_) for l_ in range(nl)]
        self.xa_wo_b = [Tile(dt("xa_wo_b%d" % l_, [D, D], BF16, kind="Internal").ap(), "xa_wo_b%d" % l_) for l_ in range(nl)]
        self.peer_wq_d = dt("peer_wq", [nl, D, 2048], F32, kind="ExternalInput").ap()
        self.keysT_d = dt("keysT", [nl, 128, 16 * 128], F32, kind="ExternalInput").ap()
        self.UT_d = dt("UT", [nl, NUG, 128, DC * UG], F32, kind="ExternalInput").ap()
        self.V_d = dt("V", [nl, NEXP, D], F32, kind="ExternalInput").ap()
        self.peer_wq_b = [Tile(dt("peer_wq_b%d" % l_, [D, 2048], BF16, kind="Internal").ap(), "peer_wq_b%d" % l_) for l_ in range(nl)]
        self.keysT_b = [Tile(dt("keysT_b%d" % l_, [128, 16 * 128], BF16, kind="Internal").ap(), "keysT_b%d" % l_) for l_ in range(nl)]
        self.UV_b = [Tile(dt("UV_b%d" % l, [NUG, 128, 4096], BF16, kind="Internal").ap(), "UV_b%d" % l) for l in range(nl)]
        self.w_in_b = [Tile(dt("w_in_b%d" % l_, [D, INC], BF16, kind="Internal").ap(), "w_in_b%d" % l_) for l_ in range(nl)]
        self.w_out_b = [Tile(dt("w_out_b%d" % l_, [D, D], BF16, kind="Internal").ap(), "w_out_b%d" % l_) for l_ in range(nl)]
        if cfg.get("dbg"):
            self.dbg_ret = dt("dbg_ret", [128, NT, 512], BF16, kind="ExternalOutput").ap()
            self.dbg_sb = dt("dbg_sb", [128, NT, 512], BF16, kind="ExternalOutput").ap()
        sb = lambda name, shape, dtp: stack.enter_context(nc.sbuf_tensor(name, shape, dtp))
        xs = sb("xs", [128, NT, D], F32)
        self.x = [Tile(xs[:, t, :], "x%d" % t) for t in range(NT)]
        self.ident_f = Tile(sb("ident_f", [128, 128], F32)[:], "ident_f")
        self.ident_b = Tile(sb("ident_b", [128, 128], BF16)[:], "ident_b")
        self.ones_b = Tile(sb("ones_b", [128, 128], BF16)[:], "ones_b")
        self.tri_incl = Tile(sb("tri_incl", [128, 128], BF16)[:], "tri_incl")
        self.tri_low = Tile(sb("tri_low", [128, 128], BF16)[:], "tri_low")
        self.gcols = Tile(sb("gcols_sb", [128, 40 * nl], F32)[:], "gcols")
        self.iota_i = Tile(sb("iota_i", [128, 128], I32)[:], "iota_i")
        self.iota_f = Tile(sb("iota_f", [128, 128], F32)[:], "iota_f")
        self.iota_b = Tile(sb("iota_b", [128, 128], BF16)[:], "iota_b")
        self.dq = Tile(sb("dq_sb", [128, NT, 4], F32)[:], "dq")
        self.dk = Tile(sb("dk_sb", [128, NT, 4], F32)[:], "dk")
        self.ARENA = 136 * KB
        self.arena = sb("arena", [128, self.ARENA // 2], BF16)
        self.banks = [stack.enter_context(nc.psum_tensor("bank%d" % i, [128, 512], F32)) for i in range(8)]

    def region(self, lo_kb, hi_kb):
        return Region(self.arena, int(lo_kb * KB), int(hi_kb * KB))

    def psum(self, i, name=""):
        return Tile(self.banks[i][:], name or ("bank%d" % i))

    def psum_bf(self, i, name=""):
        return Tile(self.banks[i][:].bitcast(BF16), name or ("bankbf%d" % i))

    def setup(self):
        k, nc = self.k, self.nc
        k.op(k.pool, lambda: nc.gpsimd.memset(self.ident_f[:], 0.0), W=[self.ident_f])
        k.op(k.pool, lambda: nc.gpsimd.affine_select(out=self.ident_f[:], in_=self.ident_f[:], pattern=[[-1, 128]],
                                                     compare_op=ALU.not_equal, fill=1.0, base=0, channel_multiplier=1),
             R=[self.ident_f], W=[self.ident_f])
        k.op(k.dve, lambda: nc.vector.tensor_copy(out=self.ident_b[:], in_=self.ident_f[:]), R=[self.ident_f], W=[self.ident_b])
        k.op(k.pool, lambda: nc.gpsimd.memset(self.ones_b[:], 1.0), W=[self.ones_b])
        k.op(k.pool, lambda: nc.gpsimd.memset(self.tri_incl[:], 1.0), W=[self.tri_incl])
        k.op(k.pool, lambda: nc.gpsimd.affine_select(out=self.tri_incl[:], in_=self.tri_incl[:], pattern=[[-1, 128]],
                                                     compare_op=ALU.is_ge, fill=0.0, base=0, channel_multiplier=1),
             R=[self.tri_incl], W=[self.tri_incl])
        k.op(k.pool, lambda: nc.gpsimd.memset(self.tri_low[:], 1.0), W=[self.tri_low])
        k.op(k.pool, lambda: nc.gpsimd.affine_select(out=self.tri_low[:], in_=self.tri_low[:], pattern=[[1, 128]],
                                                     compare_op=ALU.is_gt, fill=0.0, base=0, channel_multiplier=-1),
             R=[self.tri_low], W=[self.tri_low])
        k.op(k.pool, lambda: nc.gpsimd.iota(self.iota_i[:], pattern=[[1, 128]], base=0, channel_multiplier=0), W=[self.iota_i])
        k.op(k.dve, lambda: nc.vector.tensor_copy(out=self.iota_f[:], in_=self.iota_i[:]), R=[self.iota_i], W=[self.iota_f])
        k.op(k.dve, lambda: nc.vector.tensor_copy(out=self.iota_b[:], in_=self.iota_i[:]), R=[self.iota_i], W=[self.iota_b])
        for t, d in ((self.gcols, self.gcols_d), (self.dq, self.dq_d), (self.dk, self.dk_d)):
            k.dma(k.sp, t[:], d, W=[t], semtile=self.gcols)
        for l in range(self.nl):
            for r0 in range(0, D, 256):
                k.dma(k.pool, self.w_in_b[l][r0:r0 + 256, :], self.w_in_d[l, r0:r0 + 256, :], W=[self.w_in_b[l]])
            for r0 in range(0, D, 512):
                k.dma(k.pool, self.w_out_b[l][r0:r0 + 512, :], self.w_out_d[l, r0:r0 + 512, :], W=[self.w_out_b[l]])
            for r0 in range(0, D, 512):
                k.dma(k.pool, self.xa_wq_b[l][r0:r0 + 512, :], self.xa_wq_d[l, r0:r0 + 512, :], W=[self.xa_wq_b[l]])
            for r0 in range(0, D, 256):
                k.dma(k.pool, self.xa_wkv_b[l][r0:r0 + 256, :], self.xa_wkv_d[l, r0:r0 + 256, :], W=[self.xa_wkv_b[l]])
            for r0 in range(0, D, 512):
                k.dma(k.pool, self.xa_wo_b[l][r0:r0 + 512, :], self.xa_wo_d[l, r0:r0 + 512, :], W=[self.xa_wo_b[l]])
            if "peer" in self.cfg["phases"]:
                for r0 in range(0, D, 256):
                    k.dma(k.pool, self.peer_wq_b[l][r0:r0 + 256, :], self.peer_wq_d[l, r0:r0 + 256, :], W=[self.peer_wq_b[l]])
                k.dma(k.pool, self.keysT_b[l][:], self.keysT_d[l], W=[self.keysT_b[l]])
                for g0 in range(0, NUG, 8):
                    k.dma(k.pool, self.UV_b[l][g0:g0 + 8, :, 0:2048].rearrange("g p n -> p g n"),
                          self.UT_d[l, g0:g0 + 8].rearrange("g p n -> p g n"), W=[self.UV_b[l]])
                    for cc in range(2):
                        k.dma(k.pool, self.UV_b[l][g0:g0 + 8, :, 2048 + cc * 1024:2048 + (cc + 1) * 1024].rearrange("g j d -> j g d"),
                              self.V_d[l, g0 * UG:(g0 + 8) * UG, :].rearrange("(g cc j) d -> j g cc d", g=8, j=128, cc=2)[:, :, cc, :],
                              W=[self.UV_b[l]])

    def load_x(self, b):
        k = self.k
        for t in range(NT):
            k.dma(k.sp, self.x[t][:], self.x_d[b, t * 128:(t + 1) * 128, :], W=[self.x[t]])

    def store(self, b, dst):
        k = self.k
        for t in range(NT):
            k.dma(k.sp, dst[b, t * 128:(t + 1) * 128, :], self.x[t][:], R=[self.x[t]])

    def norm_act(self, src_tile, src_ap, wk):
        k, nc = self.k, self.nc
        junk, ss, lnv, rstd, hn = wk
        k.op(k.act, lambda: nc.scalar.activation(out=junk[:], in_=src_ap, func=AF.Square, accum_out=ss[:]),
             R=[src_tile], W=[junk, ss])
        k.op(k.act, lambda: nc.scalar.activation(out=lnv[:], in_=ss[:], func=AF.Ln, scale=1.0 / D, bias=self.eps_t[:]),
             R=[ss, self.eps_t], W=[lnv])
        k.op(k.act, lambda: nc.scalar.activation(out=rstd[:], in_=lnv[:], func=AF.Exp, scale=-0.5), R=[lnv], W=[rstd])
        k.op(k.act, lambda: nc.scalar.activation(out=hn[:], in_=src_ap, func=AF.Copy, scale=rstd[:]),
             R=[src_tile, rstd], W=[hn])

    def norm_tr(self, gbase, dst_tile, dst_ap3, wk, bank):
        k, nc = self.k, self.nc
        hn = wk[4]
        pT = self.ps[bank]
        for c in range(DC):
            k.op(k.pe, lambda c=c: nc.tensor.transpose(out=pT.bf[:, c * 128:(c + 1) * 128], in_=hn[:, c * 128:(c + 1) * 128],
                                                       identity=self.ident_b[:]),
                 R=[hn, self.ident_b], W=[pT], sig=(c == DC - 1))
        gc = self.gcols[:, gbase:gbase + DC].unsqueeze(2).broadcast_to([128, DC, 128])
        k.op(k.dve, lambda: nc.vector.tensor_tensor(out=dst_ap3, in0=pT.bf.rearrange("p (c t) -> p c t", c=DC), in1=gc,
                                                    op=ALU.mult),
             R=[pT, self.gcols], W=[dst_tile])

    def norm_T(self, src_tile, src_ap, gbase, dst_tile, dst_ap3, wk, bank):
        self.norm_act(src_tile, src_ap, wk)
        self.norm_tr(gbase, dst_tile, dst_ap3, wk, bank)

    def new_phase(self):
        self.k.barrier()
        self.ps = []
        for i in range(8):
            t = Tile(self.banks[i][:], "bank%d" % i, excl=True)
            t.bf = self.banks[i][:].bitcast(BF16)
            self.ps.append(t)

    def mixer(self, l):
        k, nc = self.k, self.nc
        gb = 40 * l
        R0 = self.region(0, 32)
        R1 = self.region(32, 80)
        R2 = self.region(80, 96)
        R3 = self.region(96, 112)
        R4 = self.region(112, 122)
        R5 = self.region(122, 130)
        self.new_phase()
        self.cos = R5.alloc([NT, 64], F32, "cos")
        self.sin = R5.alloc([NT, 64], F32, "sin")
        k.dma(k.sp, self.cos[:], self.cos_d, W=[self.cos])
        k.dma(k.sp, self.sin[:], self.sin_d, W=[self.sin])
        hT = [R0.alloc([DC, 512], BF16, "hT%d" % g) for g in range(4)]
        R4.reset()
        self.eps_t = R4.alloc([1], F32, "eps")
        k.op(k.dve, lambda: nc.vector.memset(self.eps_t[:], EPS), W=[self.eps_t])
        wks = [(R4.alloc([D], BF16, "junk"), R4.alloc([1], F32, "ss"), R4.alloc([1], F32, "lnv"), R4.alloc([1], F32, "rstd"),
                R4.alloc([D], BF16, "hn")) for _ in range(2)]
        self.norm_act(self.x[0], self.x[0][:], wks[0])
        for t in range(NT):
            g, tl = divmod(t, 4)
            if t + 1 < NT:
                self.norm_act(self.x[t + 1], self.x[t + 1][:], wks[(t + 1) % 2])
            self.norm_tr(gb + 0, hT[g], hT[g][:, :, tl * 128:(tl + 1) * 128], wks[t % 2], t % 2)
        if self.cfg.get('mx_stop', 99) <= 1:
            return
        self.new_phase()
        R4.reset()
        rqT = R1.alloc([4, S], BF16, "rqT")
        rkT = R1.alloc([4, S], BF16, "rkT")
        r_v = R1.alloc([NT, 512], BF16, "r_v")
        ret_n = R2.alloc([NT, 512], BF16, "ret_n")
        wbuf = [R3.alloc([DC, 512], BF16, "wbuf%d" % i) for i in range(2)]
        nwb = [0]

        def load_w(col0):
            wb = wbuf[nwb[0] % 2]
            nwb[0] += 1
            k.dma(k.sp, wb[:], self.w_in_b[l][:, col0:col0 + 512].rearrange("(c p) n -> p c n", p=128), R=[self.w_in_b[l]], W=[wb])
            return wb

        def proj_tm(wb, t, bank):
            g, tl = divmod(t, 4)
            ps = self.ps[bank]
            for c in range(DC):
                k.op(k.pe, lambda c=c: nc.tensor.matmul(ps[:], lhsT=hT[g][:, c, tl * 128:(tl + 1) * 128], rhs=wb[:, c, :],
                                                        start=(c == 0), stop=(c == DC - 1)),
                     R=[hT[g], wb], W=[ps], sig=(c == DC - 1))
            return ps

        rot_tmp = [R4.alloc([4, 2, 64], F32, "rot_tmp%d" % i) for i in range(2)]
        rot_a = R4.alloc([4, 64], F32, "rot_a")
        rot_b = R4.alloc([4, 64], F32, "rot_b")
        rot_o = [R4.alloc([4, 128], BF16, "rot_o%d" % i) for i in range(2)]
        def emit_tr(t, dstT):
            ro = rot_o[t % 2]
            pT = self.ps[4 + t % 2]
            for h in range(4):
                k.op(k.pe, lambda h=h: nc.tensor.transpose(out=pT.bf[:, h * 128:(h + 1) * 128], in_=ro[:, h, :],
                                                           identity=self.ident_b[:]),
                     R=[ro, self.ident_b], W=[pT], sig=(h == 3))
            k.op(k.act, lambda: nc.scalar.copy(out=dstT[:, :, t * 128:(t + 1) * 128],
                                               in_=pT.bf[:, 0:512].rearrange("p (h t) -> p h t", h=4)),
                 R=[pT], W=[dstT])

        for which, (col0, dstT, dec) in enumerate(((1536, rqT, self.dq), (2048, rkT, self.dk))):
            wb = load_w(col0)
            pss = {0: proj_tm(wb, 0, 2)}
            for t in range(NT):
                if t + 1 < NT:
                    pss[t + 1] = proj_tm(wb, t + 1, 2 + (t + 1) % 2)
                ps = pss.pop(t)
                v = ps[:].rearrange("p (h two d) -> p h two d", h=4, two=2)
                x1, x2 = v[:, :, 0, :], v[:, :, 1, :]
                cs = self.cos[:, t, :].unsqueeze(1).broadcast_to([128, 4, 64])
                sn = self.sin[:, t, :].unsqueeze(1).broadcast_to([128, 4, 64])
                tmp = rot_tmp[t % 2]
                ro = rot_o[t % 2]
                tt = nc.vector.tensor_tensor
                k.op(k.dve, lambda: tt(out=rot_a[:], in0=x1, in1=cs, op=ALU.mult), R=[ps, self.cos], W=[rot_a])
                k.op(k.dve, lambda: tt(out=rot_b[:], in0=x2, in1=sn, op=ALU.mult), R=[ps, self.sin], W=[rot_b])
                k.op(k.dve, lambda: tt(out=tmp[:, :, 0, :], in0=rot_a[:], in1=rot_b[:], op=ALU.subtract), R=[rot_a, rot_b], W=[tmp])
                k.op(k.dve, lambda: tt(out=rot_a[:], in0=x1, in1=sn, op=ALU.mult), R=[ps, self.sin], W=[rot_a])
                k.op(k.dve, lambda: tt(out=rot_b[:], in0=x2, in1=cs, op=ALU.mult), R=[ps, self.cos], W=[rot_b])
                k.op(k.dve, lambda: tt(out=tmp[:, :, 1, :], in0=rot_a[:], in1=rot_b[:], op=ALU.add), R=[rot_a, rot_b], W=[tmp])
                dcb = dec[:, t, :].unsqueeze(2).broadcast_to([128, 4, 128])
                k.op(k.dve, lambda: tt(out=ro[:], in0=tmp[:].rearrange("p h two d -> p h (two d)"), in1=dcb, op=ALU.mult),
                     R=[tmp, dec], W=[ro])
                if t >= 1:
                    emit_tr(t - 1, dstT)
            emit_tr(NT - 1, dstT)
        wb = load_w(2560)
        for t in range(NT):
            ps = proj_tm(wb, t, 2 + t % 2)
            k.op(k.act, lambda: nc.scalar.copy(out=r_v[:, t, :], in_=ps[:]), R=[ps], W=[r_v])
        if self.cfg.get('mx_stop', 99) <= 2:
            return
        self.new_phase()
        R4.reset()
        ATs = [R4.alloc([512], BF16, "AT%d" % i) for i in range(3)]
        sq = [R4.alloc([4, 128], F32, "sq%d" % i) for i in range(1)]
        cen = R4.alloc([4, 128], F32, "cen")
        st = {nm: R4.alloc([4], F32, nm) for nm in ("s1", "ssq", "mean", "m2", "var", "lnv", "rstd")}
        eps4 = R4.alloc([1], F32, "eps4")
        k.op(k.dve, lambda: nc.vector.memset(eps4[:], EPS), W=[eps4])
        free_slots = [2, 1, 0]

        def ret_chain(h, tg):
            def gen():
                i = free_slots.pop()
                ST, OUT, AT = self.ps[i], self.ps[3 + i], ATs[i]
                k.op(k.dve, lambda: nc.vector.memset(OUT[:], 0.0), W=[OUT])
                yield
                for c in range(4 * tg + 3, -1, -1):
                    t0 = max(512 * tg, 128 * c)
                    w = 512 * (tg + 1) - t0
                    off = t0 - 512 * tg
                    k.op(k.pe, lambda: nc.tensor.matmul(ST[:, 0:w], lhsT=rkT[:, h, c * 128:(c + 1) * 128], rhs=rqT[:, h, t0:t0 + w],
                                                        start=True, stop=True), R=[rkT, rqT], W=[ST])
                    yield
                    k.op(k.act, lambda: nc.scalar.copy(out=AT[:, 0:w], in_=ST[:, 0:w]), R=[ST], W=[AT])
                    if c >= 4 * tg:
                        k.op(k.pool, lambda: nc.gpsimd.affine_select(out=AT[:, 0:128], in_=AT[:, 0:128], pattern=[[1, 128]],
                                                                     compare_op=ALU.is_ge, fill=0.0, base=0, channel_multiplier=-1),
                             R=[AT], W=[AT])
                    yield
                    nq = w // 128
                    for j in range(nq):
                        qbl = off // 128 + j
                        k.op(k.pe, lambda j=j, qbl=qbl: nc.tensor.matmul(OUT[:, qbl * 128:(qbl + 1) * 128], lhsT=AT[:, j * 128:(j + 1) * 128],
                                                                         rhs=r_v[:, c, h * 128:(h + 1) * 128], start=False, stop=False,
                                                                         skip_group_check=True),
                             R=[AT, r_v], W=[OUT], sig=(j == nq - 1))
                    yield
                o3 = OUT[:].rearrange("p (q d) -> p q d", q=4)
                k.op(k.dve, lambda: nc.vector.tensor_reduce(out=st["s1"][:], in_=o3, axis=AX.X, op=ALU.add), R=[OUT], W=[st["s1"]])
                k.op(k.act, lambda: nc.scalar.activation(out=sq[0][:], in_=o3, func=AF.Square), R=[OUT], W=[sq[0]])
                k.op(k.dve, lambda: nc.vector.tensor_reduce(out=st["ssq"][:], in_=sq[0][:], axis=AX.X, op=ALU.add), R=[sq[0]], W=[st["ssq"]])
                k.op(k.dve, lambda: nc.vector.tensor_scalar(out=st["mean"][:], in0=st["s1"][:], scalar1=1.0 / 128, scalar2=None, op0=ALU.mult),
                     R=[st["s1"]], W=[st["mean"]])
                k.op(k.dve, lambda: nc.vector.tensor_tensor(out=st["m2"][:], in0=st["mean"][:], in1=st["mean"][:], op=ALU.mult),
                     R=[st["mean"]], W=[st["m2"]])
                k.op(k.dve, lambda: nc.vector.scalar_tensor_tensor(out=st["var"][:], in0=st["ssq"][:], scalar=1.0 / 128, in1=st["m2"][:],
                                                                   op0=ALU.mult, op1=ALU.subtract),
                     R=[st["ssq"], st["m2"]], W=[st["var"]])
                k.op(k.act, lambda: nc.scalar.activation(out=st["lnv"][:], in_=st["var"][:], func=AF.Ln, bias=eps4[:]),
                     R=[st["var"], eps4], W=[st["lnv"]])
                k.op(k.act, lambda: nc.scalar.activation(out=st["rstd"][:], in_=st["lnv"][:], func=AF.Exp, scale=-0.5),
                     R=[st["lnv"]], W=[st["rstd"]])
                k.op(k.dve, lambda: nc.vector.tensor_tensor(out=cen[:], in0=o3, in1=st["mean"][:].unsqueeze(2).broadcast_to([128, 4, 128]),
                                                            op=ALU.subtract), R=[OUT, st["mean"]], W=[cen])
                k.op(k.dve, lambda: nc.vector.tensor_tensor(out=ret_n[:, 4 * tg:4 * tg + 4, h * 128:(h + 1) * 128], in0=cen[:],
                                                            in1=st["rstd"][:].unsqueeze(2).broadcast_to([128, 4, 128]), op=ALU.mult),
                     R=[cen, st["rstd"]], W=[ret_n])
                free_slots.append(i)
                yield
            return gen

        run_chains([ret_chain(h, tg) for h in range(4) for tg in range(4)], 3)
        if self.cfg.get('mx_stop', 99) <= 3:
            return
        self.new_phase()
        R4.reset()
        sg = [R4.alloc([512], F32, "sg%d" % i) for i in range(2)]
        wb = load_w(3072)
        for t in range(NT):
            ps = proj_tm(wb, t, t % 2)
            k.op(k.act, lambda: nc.scalar.activation(out=sg[t % 2][:], in_=ps[:], func=AF.Silu), R=[ps], W=[sg[t % 2]])
            k.op(k.dve, lambda: nc.vector.tensor_tensor(out=ret_n[:, t, :], in0=ret_n[:, t, :], in1=sg[t % 2][:], op=ALU.mult),
                 R=[ret_n, sg[t % 2]], W=[ret_n])
        if self.cfg.get('mx_stop', 99) <= 4:
            return
        self.new_phase()
        R1.reset()
        qT2 = R1.alloc([4, S], BF16, "qT2")
        kT2 = R1.alloc([4, S], BF16, "kT2")
        v_sb = R1.alloc([NT, 512], BF16, "v_sb")
        nev = [0]
        for col0, dstT in ((0, qT2), (512, kT2)):
            wb = load_w(col0)
            for p in range(4):
                for g in range(4):
                    ps = self.ps[nev[0] % 4]
                    for c in range(DC):
                        k.op(k.pe, lambda c=c: nc.tensor.matmul(ps[:], lhsT=wb[:, c, p * 128:(p + 1) * 128], rhs=hT[g][:, c, :],
                                                                start=(c == 0), stop=(c == DC - 1)),
                             R=[hT[g], wb], W=[ps], sig=(c == DC - 1))
                    if nev[0] % 2 == 0:
                        k.op(k.act, lambda: nc.scalar.copy(out=dstT[:, p, g * 512:(g + 1) * 512], in_=ps[:]), R=[ps], W=[dstT])
                    else:
                        k.op(k.dve, lambda: nc.vector.tensor_copy(out=dstT[:, p, g * 512:(g + 1) * 512], in_=ps[:]), R=[ps], W=[dstT])
                    nev[0] += 1
        wb = load_w(1024)
        for t in range(NT):
            ps = proj_tm(wb, t, 4 + t % 2)
            k.op(k.act, lambda: nc.scalar.copy(out=v_sb[:, t, :], in_=ps[:]), R=[ps], W=[v_sb])
        if self.cfg.get('mx_stop', 99) <= 5:
            return
        self.new_phase()
        R3.reset()
        R0.reset()
        R4.reset()
        sb_n = R3.alloc([NT, 512], BF16, "sb_n")
        eT = [[R0.alloc([512], F32, "eT") for _ in range(2)] for _ in range(3)]
        PT = [[R0.alloc([512], BF16, "PT") for _ in range(2)] for _ in range(3)]
        E2 = [[R0.alloc([512], F32, "E2") for _ in range(1)] for _ in range(3)]
        AT2 = [[R0.alloc([512], BF16, "AT2") for _ in range(1)] for _ in range(3)]
        sq2 = [R4.alloc([4, 64], F32, "sq2") for _ in range(3)]
        st2 = [{nm: R4.alloc([4], F32, nm) for nm in ("ssq", "lnv", "rstd")} for _ in range(3)]
        eps5 = R4.alloc([1], F32, "eps5")
        k.op(k.dve, lambda: nc.vector.memset(eps5[:], EPS), W=[eps5])
        free_slots = [2, 1, 0]
        out_views = [(self.ps[6], 0), (self.ps[6], 256), (self.ps[7], 0)]

        def sb_chain(h, tg):
            def gen():
                i = free_slots.pop()
                ZTs, ACC = [self.ps[i], self.ps[i]], self.ps[3 + i]
                OUT, oo = out_views[i]
                pr, pb = divmod(h, 2)
                pb *= 64
                units = list(range(4 * tg + 3, -1, -1))

                def geom(c):
                    t0 = max(512 * tg, 128 * c)
                    return t0, 512 * (tg + 1) - t0, t0 - 512 * tg

                def front(n):
                    c = units[n]
                    t0, w, off = geom(c)
                    ZT, e_, P_ = ZTs[n % 2], eT[i][n % 2], PT[i][n % 2]
                    k.op(k.pe, lambda: nc.tensor.matmul(ZT[:, 0:w], lhsT=kT2[pb:pb + 64, pr, c * 128:(c + 1) * 128],
                                                        rhs=qT2[pb:pb + 64, pr, t0:t0 + w], start=True, stop=True),
                         R=[kT2, qT2], W=[ZT])
                    yield
                    k.op(k.act, lambda: nc.scalar.activation(out=e_[:, 0:w], in_=ZT[:, 0:w], func=AF.Exp, scale=0.125), R=[ZT], W=[e_])
                    if c >= 4 * tg:
                        k.op(k.pool, lambda: nc.gpsimd.affine_select(out=e_[:, 0:128], in_=e_[:, 0:128], pattern=[[1, 128]],
                                                                     compare_op=ALU.is_gt, fill=0.0, base=0, channel_multiplier=-1),
                             R=[e_], W=[e_])
                    yield
                    k.op(k.act, lambda: nc.scalar.activation(out=P_[:, 0:w], in_=e_[:, 0:w], func=AF.Ln, bias=1.0), R=[e_], W=[P_])
                    yield

                def back(n):
                    c = units[n]
                    t0, w, off = geom(c)
                    e_, P_, E2_, A_ = eT[i][n % 2], PT[i][n % 2], E2[i][0], AT2[i][0]
                    k.op(k.pe, lambda: nc.tensor.matmul(ACC[:, off:off + w], lhsT=self.tri_incl[:], rhs=P_[:, 0:w], start=False, stop=False,
                                                        skip_group_check=True), R=[self.tri_incl, P_], W=[ACC])
                    yield
                    k.op(k.act, lambda: nc.scalar.activation(out=E2_[:, 0:w], in_=ACC[:, off:off + w], func=AF.Exp, scale=-1.0),
                         R=[ACC], W=[E2_])
                    yield
                    k.op(k.pe, lambda: nc.tensor.matmul(ACC[:, off:off + w], lhsT=self.tri_low[:], rhs=P_[:, 0:w], start=False, stop=False,
                                                        skip_group_check=True), R=[self.tri_low, P_], W=[ACC])
                    k.op(k.dve, lambda: nc.vector.tensor_tensor(out=A_[:, 0:w], in0=e_[:, 0:w], in1=E2_[:, 0:w], op=ALU.mult),
                         R=[e_, E2_], W=[A_])
                    yield
                    nq = w // 128
                    for j in range(nq):
                        qbl = off // 128 + j
                        k.op(k.pe, lambda j=j, qbl=qbl: nc.tensor.matmul(OUT[:, oo + qbl * 64:oo + (qbl + 1) * 64], lhsT=A_[:, j * 128:(j + 1) * 128],
                                                                         rhs=v_sb[:, c, h * 64:(h + 1) * 64], start=False, stop=False,
                                                                         skip_group_check=True),
                             R=[A_, v_sb], W=[OUT], sig=(j == nq - 1))
                    yield

                k.op(k.dve, lambda: nc.vector.memset(ACC[:], 0.0), W=[ACC])
                k.op(k.dve, lambda: nc.vector.memset(OUT[:, oo:oo + 256], 0.0), W=[OUT])
                yield
                yield from front(0)
                for n in range(len(units)):
                    if n + 1 < len(units):
                        yield from front(n + 1)
                    yield from back(n)
                o3 = OUT[:, oo:oo + 256].rearrange("p (q d) -> p q d", q=4)
                sq_, s_ = sq2[i], st2[i]
                k.op(k.act, lambda: nc.scalar.activation(out=sq_[:], in_=o3, func=AF.Square), R=[OUT], W=[sq_])
                k.op(k.dve, lambda: nc.vector.tensor_reduce(out=s_["ssq"][:], in_=sq_[:], axis=AX.X, op=ALU.add), R=[sq_], W=[s_["ssq"]])
                k.op(k.act, lambda: nc.scalar.activation(out=s_["lnv"][:], in_=s_["ssq"][:], func=AF.Ln, scale=1.0 / 64, bias=eps5[:]),
                     R=[s_["ssq"], eps5], W=[s_["lnv"]])
                k.op(k.act, lambda: nc.scalar.activation(out=s_["rstd"][:], in_=s_["lnv"][:], func=AF.Exp, scale=-0.5),
                     R=[s_["lnv"]], W=[s_["rstd"]])
                k.op(k.dve, lambda: nc.vector.tensor_tensor(out=sb_n[:, 4 * tg:4 * tg + 4, h * 64:(h + 1) * 64], in0=o3,
                                                            in1=s_["rstd"][:].unsqueeze(2).broadcast_to([128, 4, 64]), op=ALU.mult),
                     R=[OUT, s_["rstd"]], W=[sb_n])
                free_slots.append(i)
                yield
            return gen

        run_chains([sb_chain(h, tg) for h in range(8) for tg in range(4)], 3)
        if self.cfg.get('mx_stop', 99) <= 6:
            return
        self.new_phase()
        R1.reset()
        R4.reset()
        if self.cfg.get("dbg"):
            k.dma(k.sp, self.dbg_ret, ret_n[:], R=[ret_n])
            k.dma(k.sp, self.dbg_sb, sb_n[:], R=[sb_n])
        wo = R1.alloc([DC, D], BF16, "wo")
        k.dma(k.sp, wo[:], self.w_out_b[l][:].rearrange("(c p) n -> p c n", p=128), R=[self.w_out_b[l]], W=[wo])
        mixT = [R4.alloc([DC, 128], BF16, "mixT%d" % i) for i in range(2)]
        gc = self.gcols[:, gb + 32:gb + 40].unsqueeze(2).broadcast_to([128, DC, 128])

        def emit_T(t):
            pT = self.ps[t % 2]
            for c in range(DC):
                src_t, src = (sb_n, sb_n[:, t, c * 128:(c + 1) * 128]) if c < 4 else (ret_n, ret_n[:, t, (c - 4) * 128:(c - 3) * 128])
                k.op(k.pe, lambda c=c, src=src: nc.tensor.transpose(out=pT.bf[:, c * 128:(c + 1) * 128], in_=src, identity=self.ident_b[:]),
                     R=[src_t, self.ident_b], W=[pT], sig=(c == DC - 1))
            mt = mixT[t % 2]
            k.op(k.dve, lambda: nc.vector.tensor_tensor(out=mt[:], in0=pT.bf.rearrange("p (c t) -> p c t", c=DC), in1=gc, op=ALU.mult),
                 R=[pT, self.gcols], W=[mt])

        emit_T(0)
        for t in range(NT):
            if t + 1 < NT:
                emit_T(t + 1)
            mt = mixT[t % 2]
            for half in range(2):
                ps = self.ps[2 + 2 * (t % 2) + half]
                for c in range(DC):
                    k.op(k.pe, lambda c=c: nc.tensor.matmul(ps[:], lhsT=mt[:, c, :], rhs=wo[:, c, half * 512:(half + 1) * 512],
                                                            start=(c == 0), stop=(c == DC - 1)),
                         R=[mt, wo], W=[ps], sig=(c == DC - 1))
                xa = self.x[t][:, half * 512:(half + 1) * 512]
                k.op(k.dve, lambda: nc.vector.tensor_tensor(out=xa, in0=xa, in1=ps[:], op=ALU.add), R=[self.x[t], ps], W=[self.x[t]])

    def xattn(self, l, b):
        k, nc = self.k, self.nc
        gb = 40 * l
        R0 = self.region(0, 32)
        R1 = self.region(32, 80)
        R2 = self.region(80, 96)
        R3 = self.region(96, 112)
        R4 = self.region(112, 122)
        self.new_phase()
        hT = [R0.alloc([DC, 512], BF16, "hT%d" % g) for g in range(4)]
        qT = R1.alloc([DC, S], BF16, "qT")
        memT = R1.alloc([DC, NM], BF16, "memT")
        kT = R1.alloc([DC, NM], BF16, "kT")
        vv = R1.alloc([2, D], BF16, "vv")
        memx = [R2.alloc([D], F32, "memx%d" % i) for i in range(2)]
        self.eps_t = R4.alloc([1], F32, "eps")
        k.op(k.dve, lambda: nc.vector.memset(self.eps_t[:], EPS), W=[self.eps_t])
        wks = [(R4.alloc([D], BF16, "junk"), R4.alloc([1], F32, "ss"), R4.alloc([1], F32, "lnv"), R4.alloc([1], F32, "rstd"),
                R4.alloc([D], BF16, "hn")) for _ in range(2)]
        for i in range(2):
            k.dma(k.sp, memx[i][:], self.mem_d[b, i * 128:(i + 1) * 128, :], W=[memx[i]])
        for i in range(2):
            self.norm_T(memx[i], memx[i][:], gb + 16, memT, memT[:, :, i * 128:(i + 1) * 128], wks[i], i)
        self.norm_act(self.x[0], self.x[0][:], wks[0])
        for t in range(NT):
            g, tl = divmod(t, 4)
            if t + 1 < NT:
                self.norm_act(self.x[t + 1], self.x[t + 1][:], wks[(t + 1) % 2])
            self.norm_tr(gb + 8, hT[g], hT[g][:, :, tl * 128:(tl + 1) * 128], wks[t % 2], t % 2)
        wbuf = [R3.alloc([DC, 512], BF16, "wbuf%d" % i) for i in range(2)]
        nwb = [0]

        def load_w(src_tile, src_ap):
            wb = wbuf[nwb[0] % 2]
            nwb[0] += 1
            k.dma(k.sp, wb[:], src_ap.rearrange("(c p) n -> p c n", p=128), R=[src_tile], W=[wb])
            return wb

        nev = [0]

        def evac(out_ap, out_tile, ps, in_ap):
            if nev[0] % 2 == 0:
                k.op(k.act, lambda: nc.scalar.copy(out=out_ap, in_=in_ap), R=[ps], W=[out_tile])
            else:
                k.op(k.dve, lambda: nc.vector.tensor_copy(out=out_ap, in_=in_ap), R=[ps], W=[out_tile])
            nev[0] += 1

        for half in range(2):
            wb = load_w(self.xa_wkv_b[l], self.xa_wkv_b[l][:, half * 512:(half + 1) * 512])
            for j in range(4):
                ps = self.ps[2 + nev[0] % 4]
                for c in range(DC):
                    k.op(k.pe, lambda c=c: nc.tensor.matmul(ps[:, 0:NM], lhsT=wb[:, c, j * 128:(j + 1) * 128], rhs=memT[:, c, :],
                                                            start=(c == 0), stop=(c == DC - 1)), R=[wb, memT], W=[ps], sig=(c == DC - 1))
                evac(kT[:, half * 4 + j, :], kT, ps, ps[:, 0:NM])
        for half in range(2):
            wb = load_w(self.xa_wkv_b[l], self.xa_wkv_b[l][:, D + half * 512:D + (half + 1) * 512])
            for mt in range(2):
                ps = self.ps[2 + nev[0] % 4]
                for c in range(DC):
                    k.op(k.pe, lambda c=c: nc.tensor.matmul(ps[:], lhsT=memT[:, c, mt * 128:(mt + 1) * 128], rhs=wb[:, c, :],
                                                            start=(c == 0), stop=(c == DC - 1)), R=[wb, memT], W=[ps], sig=(c == DC - 1))
                evac(vv[:, mt, half * 512:(half + 1) * 512], vv, ps, ps[:])
        for half in range(2):
            wb = load_w(self.xa_wq_b[l], self.xa_wq_b[l][:, half * 512:(half + 1) * 512])
            for j in range(4):
                for g in range(4):
                    ps = self.ps[2 + nev[0] % 4]
                    for c in range(DC):
                        k.op(k.pe, lambda c=c: nc.tensor.matmul(ps[:], lhsT=wb[:, c, j * 128:(j + 1) * 128], rhs=hT[g][:, c, :],
                                                                start=(c == 0), stop=(c == DC - 1)), R=[wb, hT[g]], W=[ps], sig=(c == DC - 1))
                    evac(qT[:, half * 4 + j, g * 512:(g + 1) * 512], qT, ps, ps[:])
        self.new_phase()
        R0.reset()
        R4.reset()
        R2.reset()
        oT = [R0.alloc([DC, 512], BF16, "oT%d" % g) for g in range(4)]
        pT = [[R4.alloc([512], BF16, "pT") for _ in range(2)] for _ in range(2)]
        rec = [R4.alloc([512], F32, "rec") for _ in range(2)]
        wo = R2.alloc([DC, D], BF16, "wo")
        k.dma(k.sp, wo[:], self.xa_wo_b[l][:].rearrange("(c p) n -> p c n", p=128), R=[self.xa_wo_b[l]], W=[wo])
        it = 0
        for g in range(4):
            for h in range(4):
                pp = pT[it % 2]
                for mt in range(2):
                    ps = self.ps[(it % 2) * 2 + mt]
                    for kc in range(2):
                        k.op(k.pe, lambda kc=kc: nc.tensor.matmul(ps[:], lhsT=kT[:, 2 * h + kc, mt * 128:(mt + 1) * 128],
                                                                  rhs=qT[:, 2 * h + kc, g * 512:(g + 1) * 512], start=(kc == 0), stop=(kc == 1)),
                             R=[kT, qT], W=[ps], sig=(kc == 1))
                    k.op(k.act, lambda: nc.scalar.activation(out=pp[mt][:], in_=ps[:], func=AF.Exp, scale=1.0 / 16), R=[ps], W=[pp[mt]])
                sm = self.ps[4]
                for mt in range(2):
                    k.op(k.pe, lambda mt=mt: nc.tensor.matmul(sm[:], lhsT=self.ones_b[:], rhs=pp[mt][:], start=(mt == 0), stop=(mt == 1)),
                         R=[self.ones_b, pp[mt]], W=[sm], sig=(mt == 1))
                rc = rec[it % 2]
                k.op(k.dve, lambda: nc.vector.reciprocal(out=rc[:], in_=sm[:]), R=[sm], W=[rc])
                for dvc in range(2):
                    po = self.ps[5 + dvc]
                    for mt in range(2):
                        k.op(k.pe, lambda mt=mt: nc.tensor.matmul(po[:], lhsT=vv[:, mt, h * 256 + dvc * 128:h * 256 + (dvc + 1) * 128], rhs=pp[mt][:],
                                                                  start=(mt == 0), stop=(mt == 1)), R=[vv, pp[mt]], W=[po], sig=(mt == 1))
                    k.op(k.dve, lambda: nc.vector.tensor_tensor(out=oT[g][:, 2 * h + dvc, :], in0=po[:], in1=rc[:], op=ALU.mult),
                         R=[po, rc], W=[oT[g]])
                it += 1
        for t in range(NT):
            g, tl = divmod(t, 4)
            for half in range(2):
                ps = self.ps[(t % 2) * 2 + half]
                for c in range(DC):
                    k.op(k.pe, lambda c=c: nc.tensor.matmul(ps[:], lhsT=oT[g][:, c, tl * 128:(tl + 1) * 128], rhs=wo[:, c, half * 512:(half + 1) * 512],
                                                            start=(c == 0), stop=(c == DC - 1)), R=[oT[g], wo], W=[ps], sig=(c == DC - 1))
                xa = self.x[t][:, half * 512:(half + 1) * 512]
                k.op(k.dve, lambda: nc.vector.tensor_tensor(out=xa, in0=xa, in1=ps[:], op=ALU.add), R=[self.x[t], ps], W=[self.x[t]])


    def peer(self, l):
        k, nc = self.k, self.nc
        gb = 40 * l
        RW = self.region(0, 64)
        RS = self.region(64, 96)
        RH = self.region(96, 104)
        RP = self.region(104, 136)
        self.new_phase()
        tt = nc.vector.tensor_tensor
        W_sb = RW.alloc([TB, 128], BF16, "W_sb")
        NSB = 4
        sbuf = [RS.alloc([4096], BF16, "sbuf%d" % i) for i in range(NSB)]
        hTbs = [RH.alloc([DC, TB], BF16, "hTb%d" % i) for i in range(2)]
        nsb = [0]

        def next_sbuf():
            b_ = sbuf[nsb[0] % NSB]
            nsb[0] += 1
            return b_

        qTb = RP.alloc([16, 128], BF16, "qTb")
        self.eps_t = RP.alloc([1], F32, "eps")
        k.op(k.dve, lambda: nc.vector.memset(self.eps_t[:], EPS), W=[self.eps_t])
        hn_ = RP.alloc([D], BF16, "hn")
        wk = (hn_, RP.alloc([1], F32, "ss"), RP.alloc([1], F32, "lnv"), RP.alloc([1], F32, "rstd"), hn_)
        tops = RP.alloc([16, 16], F32, "tops")
        tidx = RP.alloc([16, 16], U32, "tidx")
        tidxf = RP.alloc([16, 16], F32, "tidxf")
        cand = RP.alloc([16, 16], F32, "cand")
        cand2 = RP.alloc([256], F32, "cand2")
        s2 = cand2
        best = RP.alloc([8, 16], F32, "best")
        pos = RP.alloc([8, 16], U32, "pos")
        k1u = RP.alloc([8, 16], U32, "k1u")
        k2u = RP.alloc([8, 16], U32, "k2u")
        k1f = RP.alloc([8, 16], F32, "k1f")
        k2f = RP.alloc([8, 16], F32, "k2f")
        eq = RP.alloc([2, 16, 16], BF16, "eq")
        prod = RP.alloc([2, 16, 16], BF16, "prod")
        ex = RP.alloc([8, 16], F32, "ex")
        ssum = RP.alloc([8], F32, "ssum")
        g_tm = RP.alloc([8, 16], F32, "g_tm")
        I_tm = RP.alloc([8, 16], F32, "I_tm")
        J_tm = RP.alloc([8, 16], F32, "J_tm")
        Ism = RP.alloc([TB], F32, "Ism")
        Jsm = RP.alloc([TB], F32, "Jsm")
        gsm = RP.alloc([TB], F32, "gsm")
        TS = 4
        OJ = [RP.alloc([TS, 128], BF16, "OJ") for _ in range(2)]
        OI = [RP.alloc([TS, 128], BF16, "OI") for _ in range(2)]
        NB = 3
        ga = [RP.alloc([TB], BF16, "ga") for _ in range(2)]
        keysT = RP.alloc([16, 128], BF16, "keysT")
        k.dma(k.sp, keysT[:], self.keysT_b[l][:].rearrange("p (g n) -> p g n", g=16), R=[self.keysT_b[l]], W=[keysT])
        G = [RP.alloc([TB], BF16, "G") for _ in range(NB)]
        OUT = [[self.ps[0], self.ps[1]], [self.ps[2], self.ps[3]]]
        PA = [self.ps[4], self.ps[5]]
        PB = [self.ps[6], self.ps[7]]

        def part1(blk):
            hTb = hTbs[blk % 2]

            def emit_tr(ti_):
                pt = PB[0]
                for j, src in enumerate((I_tm, J_tm, g_tm)):
                    k.op(k.pe, lambda j=j, src=src: nc.tensor.transpose(out=pt[:, j * 128:(j + 1) * 128], in_=src[:].rearrange("p a b -> p (a b)"),
                                                                        identity=self.ident_f[:]), R=[src, self.ident_f], W=[pt], sig=(j == 2))
                for j, dst in enumerate((Ism, Jsm, gsm)):
                    k.op(k.act, lambda j=j, dst=dst: nc.scalar.copy(out=dst[:, ti_ * 128:(ti_ + 1) * 128], in_=pt[:, j * 128:(j + 1) * 128]), R=[pt], W=[dst])

            for ti in range(2):
                t = blk * 2 + ti
                self.norm_act(self.x[t], self.x[t][:], wk)
                for _ in range(4):
                    yield
                self.norm_tr(gb + 24, hTb, hTb[:, :, ti * 128:(ti + 1) * 128], wk, 6)
                yield
            kb = keysT
            kv = keysT[:]
            for ti in range(2):
                for q4 in range(4):
                    wb = next_sbuf()
                    wv = wb[:].rearrange("p (c n) -> p c n", c=DC)
                    k.dma(k.sp, wv, self.peer_wq_b[l][:, q4 * 512:(q4 + 1) * 512].rearrange("(c p) n -> p c n", p=128), R=[self.peer_wq_b[l]], W=[wb])
                    ps = PB[1]
                    for j in range(4):
                        for c in range(DC):
                            k.op(k.pe, lambda c=c, j=j: nc.tensor.matmul(ps[:, j * 128:(j + 1) * 128], lhsT=wv[:, c, j * 128:(j + 1) * 128],
                                                                         rhs=hTb[:, c, ti * 128:(ti + 1) * 128],
                                                                         start=(c == 0), stop=(c == DC - 1)), R=[wb, hTb], W=[ps], sig=(c == DC - 1))
                        yield
                    k.op(k.act, lambda: nc.scalar.copy(out=qTb[:, q4 * 4:q4 * 4 + 4, :], in_=ps[:].rearrange("p (j t) -> p j t", j=4)), R=[ps], W=[qTb])
                    yield
                if ti == 1:
                    emit_tr(0)
                for g4 in range(4):
                    ps = PB[g4 % 2]
                    for gi in range(4):
                        gq = g4 * 4 + gi
                        k.op(k.pe, lambda gq=gq, gi=gi: nc.tensor.matmul(ps[:, gi * 128:(gi + 1) * 128], lhsT=qTb[:, gq, :],
                                                                         rhs=kv[:, gq, :], start=True, stop=True), R=[qTb, kb], W=[ps], sig=(gi == 3))
                    yield
                    for gi in range(4):
                        gq = g4 * 4 + gi
                        sc = ps[:, gi * 128:(gi + 1) * 128]
                        k.op(k.dve, lambda: nc.vector.max(out=tops[:, gq, 0:8], in_=sc), R=[ps], W=[tops])
                        k.op(k.dve, lambda: nc.vector.max_index(out=tidx[:, gq, 0:8], in_max=tops[:, gq, 0:8], in_values=sc), R=[ps, tops], W=[tidx])
                        k.op(k.dve, lambda: nc.vector.match_replace(out=s2[:, 0:128], in_to_replace=tops[:, gq, 0:8], in_values=sc, imm_value=-1e30),
                             R=[ps, tops], W=[s2])
                        yield
                        k.op(k.dve, lambda: nc.vector.max(out=tops[:, gq, 8:16], in_=s2[:, 0:128]), R=[s2], W=[tops])
                        k.op(k.dve, lambda: nc.vector.max_index(out=tidx[:, gq, 8:16], in_max=tops[:, gq, 8:16], in_values=s2[:, 0:128]), R=[s2, tops], W=[tidx])
                        yield
                k.op(k.dve, lambda: nc.vector.tensor_copy(out=tidxf[:], in_=tidx[:]), R=[tidx], W=[tidxf])
                for h in range(8):
                    k.op(k.dve, lambda: tt(out=cand[:], in0=tops[:, 2 * h, :].unsqueeze(2).broadcast_to([128, 16, 16]),
                                           in1=tops[:, 2 * h + 1, :].unsqueeze(1).broadcast_to([128, 16, 16]), op=ALU.add), R=[tops], W=[cand])
                    cf = cand[:].rearrange("p a b -> p (a b)")
                    k.op(k.dve, lambda: nc.vector.max(out=best[:, h, 0:8], in_=cf), R=[cand], W=[best])
                    k.op(k.dve, lambda: nc.vector.max_index(out=pos[:, h, 0:8], in_max=best[:, h, 0:8], in_values=cf), R=[cand, best], W=[pos])
                    yield
                    k.op(k.dve, lambda: nc.vector.match_replace(out=cand2[:], in_to_replace=best[:, h, 0:8], in_values=cf, imm_value=-1e30),
                         R=[cand, best], W=[cand2])
                    k.op(k.dve, lambda: nc.vector.max(out=best[:, h, 8:16], in_=cand2[:]), R=[cand2], W=[best])
                    k.op(k.dve, lambda: nc.vector.max_index(out=pos[:, h, 8:16], in_max=best[:, h, 8:16], in_values=cand2[:]), R=[cand2, best], W=[pos])
                    yield
                k.op(k.dve, lambda: tt(out=ex[:], in0=best[:], in1=best[:, :, 0:1].broadcast_to([128, 8, 16]), op=ALU.subtract), R=[best], W=[ex])
                k.op(k.act, lambda: nc.scalar.activation(out=ex[:], in_=ex[:], func=AF.Exp), R=[ex], W=[ex])
                k.op(k.dve, lambda: nc.vector.tensor_reduce(out=ssum[:], in_=ex[:], axis=AX.X, op=ALU.add), R=[ex], W=[ssum])
                k.op(k.dve, lambda: nc.vector.reciprocal(out=ssum[:], in_=ssum[:]), R=[ssum], W=[ssum])
                k.op(k.dve, lambda: tt(out=g_tm[:], in0=ex[:], in1=ssum[:].unsqueeze(2).broadcast_to([128, 8, 16]), op=ALU.mult), R=[ex, ssum], W=[g_tm])
                yield
                k.op(k.dve, lambda: nc.vector.tensor_single_scalar(out=k1u[:], in_=pos[:], scalar=4, op=ALU.logical_shift_right), R=[pos], W=[k1u])
                k.op(k.dve, lambda: nc.vector.tensor_single_scalar(out=k2u[:], in_=pos[:], scalar=15, op=ALU.bitwise_and), R=[pos], W=[k2u])
                k.op(k.dve, lambda: nc.vector.tensor_copy(out=k1f[:], in_=k1u[:]), R=[k1u], W=[k1f])
                k.op(k.dve, lambda: nc.vector.tensor_copy(out=k2f[:], in_=k2u[:]), R=[k2u], W=[k2f])
                yield
                io16 = self.iota_f[:, 0:16].unsqueeze(1).unsqueeze(1).broadcast_to([128, 2, 16, 16])
                tf4 = tidxf[:].rearrange("p (h two) k -> p h two k", two=2)
                for kf, half, dst in ((k1f, 0, I_tm), (k2f, 1, J_tm)):
                    for hh in range(4):
                        hs = slice(hh * 2, hh * 2 + 2)
                        k.op(k.dve, lambda: tt(out=eq[:], in0=kf[:, hs, :].unsqueeze(3).broadcast_to([128, 2, 16, 16]), in1=io16, op=ALU.is_equal),
                             R=[kf, self.iota_f], W=[eq])
                        k.op(k.dve, lambda: tt(out=prod[:], in0=eq[:], in1=tf4[:, hs, half, :].unsqueeze(2).broadcast_to([128, 2, 16, 16]), op=ALU.mult),
                             R=[eq, tidxf], W=[prod])
                        k.op(k.dve, lambda: nc.vector.tensor_reduce(out=dst[:, hs, :], in_=prod[:], axis=AX.X, op=ALU.add), R=[prod], W=[dst])
                        yield
                if ti == 1:
                    for _ in range(12):
                        yield
                    emit_tr(1)
                yield

        def part2(blk):
            iob = self.iota_f[:].unsqueeze(1).broadcast_to([128, TS, 128])
            banks = [self.ps[4], self.ps[5], self.ps[6], self.ps[7]]
            for sbi in range(TB // TS):
                t0 = sbi * TS
                oj, oi = OJ[sbi % 2], OI[sbi % 2]
                for q in range(TS):
                    tq = t0 + q
                    k.op(k.dve, lambda q=q, tq=tq: nc.vector.tensor_scalar(out=oj[:, q, :], in0=self.iota_b[:], scalar1=Jsm[:, tq:tq + 1], scalar2=None,
                                                                           op0=ALU.is_equal), R=[self.iota_b, Jsm], W=[oj], indep=True, sig=False)
                    k.op(k.dve, lambda q=q, tq=tq: nc.vector.tensor_scalar(out=oi[:, q, :], in0=self.iota_b[:], scalar1=Ism[:, tq:tq + 1],
                                                                           scalar2=gsm[:, tq:tq + 1], op0=ALU.is_equal, op1=ALU.mult),
                         R=[self.iota_b, Ism, gsm], W=[oi], indep=True, sig=(q == TS - 1))
                pw = banks[sbi % 4]
                for q in range(4):
                    k.op(k.pe, lambda q=q: nc.tensor.matmul(pw[:, q * 128:(q + 1) * 128], lhsT=oj[:, q, :], rhs=oi[:, q, :], start=True, stop=True),
                         R=[oj, oi], W=[pw], sig=(q == 3))
                k.op(k.act, lambda: nc.scalar.copy(out=W_sb[:, t0:t0 + 4, :], in_=pw[:].rearrange("p (t i) -> p t i", t=4)), R=[pw], W=[W_sb])

        def loop(blk):
            hTb = hTbs[blk % 2]
            for ts in range(2):
                for dh in range(2):
                    k.op(k.dve, lambda: nc.vector.memset(OUT[ts][dh][:], 0.0), W=[OUT[ts][dh]])
            sbfs = {}
            AHEAD = 2
            NCH = 2 * NUG
            for c in range(NCH + AHEAD):
                if c < NCH:
                    cg, cc = divmod(c, 2)
                    if cc == 0:
                        sbf = next_sbuf()
                        sbfs[cg] = sbf
                        k.dma(k.sp, sbf[:], self.UV_b[l][cg], R=[self.UV_b[l]], W=[sbf])
                    sbf = sbfs[cg]
                    utv = sbf[:, 0:2048].rearrange("p (c e) -> p c e", c=DC)
                    pa = PA[c % 2]
                    for dc in range(DC):
                        k.op(k.pe, lambda dc=dc: nc.tensor.matmul(pa[:, 0:TB], lhsT=utv[:, dc, cc * 128:(cc + 1) * 128], rhs=hTb[:, dc, :],
                                                                  start=(dc == 0), stop=(dc == DC - 1)), R=[sbf, hTb], W=[pa], sig=(dc == DC - 1))
                    ga_, G_ = ga[c % 2], G[c % NB]
                    k.op(k.act, lambda: nc.scalar.activation(out=ga_[:], in_=pa[:, 0:TB], func=AF.Gelu), R=[pa], W=[ga_])
                    k.op(k.pool, lambda: nc.gpsimd.tensor_tensor(out=G_[:], in0=ga_[:], in1=W_sb[:, :, c], op=ALU.mult), R=[ga_, W_sb], W=[G_])
                c2 = c - AHEAD
                if c2 >= 0:
                    cg2, cc2 = divmod(c2, 2)
                    sbf2 = sbfs[cg2]
                    vgv = sbf2[:, 2048:4096].rearrange("p (cc d) -> p cc d", cc=2)
                    G2 = G[c2 % NB]
                    for ts in range(2):
                        for dh in range(2):
                            k.op(k.pe, lambda ts=ts, dh=dh: nc.tensor.matmul(OUT[ts][dh][:], lhsT=G2[:, ts * 128:(ts + 1) * 128],
                                                                             rhs=vgv[:, cc2, dh * 512:(dh + 1) * 512], start=False, stop=False,
                                                                             skip_group_check=True),
                                 R=[G2, sbf2], W=[OUT[ts][dh]], sig=(ts == 1 and dh == 1))
                yield
            for ts in range(2):
                t = blk * 2 + ts
                for dh in range(2):
                    xa = self.x[t][:, dh * 512:(dh + 1) * 512]
                    k.op(k.dve, lambda: tt(out=xa, in0=xa, in1=OUT[ts][dh][:], op=ALU.add), R=[self.x[t], OUT[ts][dh]], W=[self.x[t]])
            yield

        def thin2(gen):
            i_ = 0
            for _ in gen:
                i_ += 1
                if i_ % 3 != 0:
                    yield
            yield

        nblk = S // TB
        for _ in part1(0):
            pass
        part2(0)
        for blk in range(nblk):
            chains = [lambda blk=blk: loop(blk)]
            if blk + 1 < nblk:
                chains.append(lambda blk=blk: thin2(part1(blk + 1)))
            run_chains(chains, 2)
            if blk + 1 < nblk:
                part2(blk + 1)

    def final(self, b):
        k, nc = self.k, self.nc
        self.new_phase()
        R = self.region(0, 32)
        fing = R.alloc([D], F32, "fing")
        k.dma(k.sp, fing[:], self.fing_d, W=[fing])
        eps = R.alloc([1], F32, "eps")
        k.op(k.dve, lambda: nc.vector.memset(eps[:], EPS), W=[eps])
        junk = R.alloc([D], BF16, "junk")
        ys = [R.alloc([D], F32, "y%d" % i) for i in range(2)]
        st = [(R.alloc([1], F32, "ss"), R.alloc([1], F32, "lnv"), R.alloc([1], F32, "rstd")) for _ in range(2)]
        for t in range(NT):
            ss, lnv, rstd = st[t % 2]
            y = ys[t % 2]
            xt = self.x[t]
            k.op(k.act, lambda: nc.scalar.activation(out=junk[:], in_=xt[:], func=AF.Square, accum_out=ss[:]), R=[xt], W=[junk, ss])
            k.op(k.act, lambda: nc.scalar.activation(out=lnv[:], in_=ss[:], func=AF.Ln, scale=1.0 / D, bias=eps[:]), R=[ss, eps], W=[lnv])
            k.op(k.act, lambda: nc.scalar.activation(out=rstd[:], in_=lnv[:], func=AF.Exp, scale=-0.5), R=[lnv], W=[rstd])
            k.op(k.dve, lambda: nc.vector.scalar_tensor_tensor(out=y[:], in0=xt[:], scalar=rstd[:], in1=fing[:], op0=ALU.mult, op1=ALU.mult),
                 R=[xt, rstd, fing], W=[y])
            k.dma(k.sp, self.out_d[b, t * 128:(t + 1) * 128, :], y[:], R=[y])


def build(cfg):
    nc = bass.Bass("TRN2", target_bir_lowering=False)
    stack = contextlib.ExitStack()
    with stack:
        b = Builder(nc, stack, cfg)
        b.new_phase()
        b.setup()
        for s in range(cfg["NSEQ"]):
            b.load_x(s)
            for l in range(cfg["L"]):
                if "mixer" in cfg["phases"]:
                    b.mixer(l)
                if "xattn" in cfg["phases"]:
                    b.xattn(l, s)
                if "peer" in cfg["phases"]:
                    b.peer(l)
            if "final" in cfg["phases"]:
                b.final(s)
            else:
                b.new_phase()
                b.store(s, b.out_d)
        b.k.barrier()
    return nc


def host_consts():
    pos = np.arange(S, dtype=np.float32)
    inv = (10000.0 ** (-np.arange(0, 128, 2, dtype=np.float32) / 128)).astype(np.float32)
    ang = pos[:, None] * inv[None, :]
    cos = np.cos(ang).astype(np.float32).reshape(NT, 128, 64).transpose(1, 0, 2)
    sin = np.sin(ang).astype(np.float32).reshape(NT, 128, 64).transpose(1, 0, 2)
    log_g = np.log1p(-np.power(2.0, -5.0 - np.arange(4, dtype=np.float64)))
    dq = np.exp(pos.astype(np.float64)[:, None] * log_g[None, :])
    dk = np.exp(-pos.astype(np.float64)[:, None] * log_g[None, :]) * (128.0 ** -0.5)
    dq = dq.astype(np.float32).reshape(NT, 128, 4).transpose(1, 0, 2)
    dk = dk.astype(np.float32).reshape(NT, 128, 4).transpose(1, 0, 2)
    return {"cos": np.ascontiguousarray(cos), "sin": np.ascontiguousarray(sin), "dq": np.ascontiguousarray(dq),
            "dk": np.ascontiguousarray(dk)}


def col_layout(v):
    return np.ascontiguousarray(np.asarray(v, np.float32).reshape(DC, 128).T)


def make_inputs(inp, cfg, ncores=1):
    nl = cfg["L"]
    nseq = cfg["NSEQ"]
    hc = host_consts()
    gcols = np.zeros((128, 40 * nl), np.float32)
    for l in range(nl):
        cat = np.concatenate([np.asarray(inp["sb_norm"][l], np.float32), np.asarray(inp["ret_norm"][l], np.float32)])
        for i, v in enumerate((inp["mix_norm"][l], inp["xa_norm"][l], inp["mem_norm"][l], inp["peer_norm"][l], cat)):
            gcols[:, 40 * l + 8 * i:40 * l + 8 * i + 8] = col_layout(v)
    shared = {
        "w_in": np.ascontiguousarray(inp["w_in"][:nl], np.float32),
        "w_out": np.ascontiguousarray(inp["w_out"][:nl], np.float32),
        "xa_wq": np.ascontiguousarray(inp["xa_wq"][:nl], np.float32),
        "xa_wkv": np.ascontiguousarray(inp["xa_wkv"][:nl], np.float32),
        "xa_wo": np.ascontiguousarray(inp["xa_wo"][:nl], np.float32),
        "peer_wq": np.ascontiguousarray(inp["peer_wq"][:nl], np.float32),
        "keysT": np.ascontiguousarray(np.transpose(np.asarray(inp["peer_keys"][:nl], np.float32), (0, 4, 1, 2, 3)).reshape(nl, 128, 2048)),
        "UT": np.ascontiguousarray(np.asarray(inp["peer_u"][:nl], np.float32).reshape(nl, NUG, UG, DC, 128).transpose(0, 1, 4, 3, 2)).reshape(nl, NUG, 128, DC * UG),
        "V": np.ascontiguousarray(inp["peer_v"][:nl], np.float32),
        "gcols": gcols,
        "fing": np.ascontiguousarray(np.broadcast_to(np.asarray(inp["final_norm"], np.float32)[None, :], (128, D))),
    }
    shared.update(hc)
    maps = []
    for c in range(ncores):
        m = dict(shared)
        m["x"] = np.ascontiguousarray(inp["x"][c * nseq:(c + 1) * nseq], np.float32)
        m["mem"] = np.ascontiguousarray(inp["mem"][c * nseq:(c + 1) * nseq], np.float32)
        maps.append(m)
    return maps


_CFG = {"L": L, "NSEQ": NSEQ, "phases": ["mixer", "xattn", "peer", "final"]}
_NC_CACHE = {}


def kernel(**inputs):
    ncores = 8
    cfg = _CFG
    if "nc" not in _NC_CACHE:
        _NC_CACHE["nc"] = build(cfg)
    nc = _NC_CACHE["nc"]
    in_maps = make_inputs(inputs, cfg, ncores=ncores)
    res = run_bass_kernel_spmd(nc, in_maps, core_ids=list(range(ncores)))
    outs = [np.asarray(r["out"]) for r in res.results]
    return np.concatenate(outs, axis=0).astype(np.float32)
```
